# Optimizing a Trainium2 kernel written in Bass

```python
import jax, jax.numpy as jnp
from jax import lax
import numpy as np

D_MODEL = 1024
BATCH = 8
SEQ = 4096
DEPTH = 1

SSD_INNER = 2 * D_MODEL
SSD_HEAD_DIM = 64
SSD_HEADS = SSD_INNER // SSD_HEAD_DIM
SSD_GROUPS = 8
SSD_HEADS_PER_GROUP = SSD_HEADS // SSD_GROUPS
SSD_STATE = 128
SSD_CONV = 4
SSD_CHUNK = 128
SSD_CONV_DIM = SSD_INNER + 2 * SSD_GROUPS * SSD_STATE
SC_WIDTH = D_MODEL
SC_KERNEL = 3
N_EXPERTS = 64
TOP_K = 8
N_EXPERT_GROUPS = 8
TOPK_EXPERT_GROUPS = 4
D_EXPERT = 256
ROUTED_SCALE = 2.5
MOE_BLOCK = 128
LN_EPS = 1e-5
RMS_EPS = 1e-5
ALPHA = (2.0 * DEPTH) ** 0.25
BETA = (8.0 * DEPTH) ** -0.25
_IN_SIZES = (SSD_INNER, SSD_CONV_DIM, SSD_HEADS, SC_WIDTH, SC_WIDTH, SC_WIDTH, D_MODEL, D_MODEL)
IN_COLS = int(sum(_IN_SIZES))
IN_SPLITS = [int(v) for v in np.cumsum(_IN_SIZES)[:-1]]

kernel_name = "hybrid_ssd_shortconv_moe_block"


def layer_norm(x):
    xf = x.astype(jnp.float32)
    mu = jnp.mean(xf, axis=-1, keepdims=True)
    xc = xf - mu
    var = jnp.mean(xc * xc, axis=-1, keepdims=True)
    return xc * lax.rsqrt(var + LN_EPS)


def causal_depthwise_conv(u, w):
    k = w.shape[0]
    return lax.conv_general_dilated(
        u, w[:, None, :].astype(u.dtype), window_strides=(1,), padding=[(k - 1, 0)],
        dimension_numbers=("NWC", "WIO", "NWC"), feature_group_count=u.shape[-1])


def gated_rmsnorm(y, z, w):
    g = y.astype(jnp.float32) * jax.nn.silu(z.astype(jnp.float32))
    g = g.reshape(*y.shape[:-1], SSD_GROUPS, -1)
    g = g * lax.rsqrt(jnp.mean(g * g, axis=-1, keepdims=True) + RMS_EPS)
    return g.reshape(y.shape) * w.astype(jnp.float32)


def ssd_chunked(xg, dt, A, Bg, Cg):
    b, s, g, r, p = xg.shape
    n = Bg.shape[-1]
    nc = s // SSD_CHUNK
    l = SSD_CHUNK
    X = (xg * dt[..., None]).reshape(b, nc, l, g, r, p)
    dA = (dt * A).reshape(b, nc, l, g, r).transpose(0, 1, 3, 4, 2)
    Bc = Bg.reshape(b, nc, l, g, n)
    Cc = Cg.reshape(b, nc, l, g, n)
    A_cs = jnp.cumsum(dA, axis=-1)
    seg = A_cs[..., :, None] - A_cs[..., None, :]
    causal = jnp.tril(jnp.ones((l, l), dtype=bool))
    Lmat = jnp.exp(jnp.where(causal, seg, -jnp.inf))
    CB = jnp.einsum("bclgn,bcsgn->bcgls", Cc, Bc)
    scores = CB[:, :, :, None] * Lmat
    y_diag = jnp.einsum("bcgrls,bcsgrp->bclgrp", scores, X)
    decay_states = jnp.exp(A_cs[..., -1:] - A_cs).transpose(0, 1, 4, 2, 3)
    states = jnp.einsum("bclgn,bclgrp->bcgrpn", Bc, X * decay_states[..., None])
    chunk_decay = jnp.exp(A_cs[..., -1])

    def step(h, inp):
        st, dec = inp
        return dec[..., None, None] * h + st, h

    h0 = jnp.zeros((b, g, r, p, n), jnp.float32)
    _, prev = lax.scan(step, h0, (states.transpose(1, 0, 2, 3, 4, 5).astype(jnp.float32),
                                  chunk_decay.transpose(1, 0, 2, 3)))
    prev = prev.transpose(1, 0, 2, 3, 4, 5)
    out_decay = jnp.exp(A_cs).transpose(0, 1, 4, 2, 3)
    y_off = jnp.einsum("bclgn,bcgrpn->bclgrp", Cc, prev) * out_decay[..., None]
    return (y_diag + y_off).reshape(b, s, g, r, p)


def hybrid_mixer(h, w_in, ssd_conv_w, ssd_conv_b, ssd_dt_bias, ssd_A_log, ssd_D, ssd_norm_w,
                 w_ssd_out, sc_conv_w, w_sc_out, w_o):
    b, s, _ = h.shape
    z, xbc, dt_raw, sc_b, sc_c, sc_h, gate_a, gate_b = jnp.split(h @ w_in, IN_SPLITS, axis=-1)
    xbc = jax.nn.silu(causal_depthwise_conv(xbc, ssd_conv_w) + ssd_conv_b)
    xs, Bm, Cm = jnp.split(xbc, [SSD_INNER, SSD_INNER + SSD_GROUPS * SSD_STATE], axis=-1)
    dt = jax.nn.softplus(dt_raw.astype(jnp.float32) + ssd_dt_bias.astype(jnp.float32))
    A = -jnp.exp(ssd_A_log.astype(jnp.float32))
    G, R = SSD_GROUPS, SSD_HEADS_PER_GROUP
    xg = xs.reshape(b, s, G, R, SSD_HEAD_DIM)
    y = ssd_chunked(xg, dt.reshape(b, s, G, R), A.reshape(G, R),
                    Bm.reshape(b, s, G, SSD_STATE), Cm.reshape(b, s, G, SSD_STATE))
    y = y + ssd_D.reshape(G, R)[:, :, None] * xg
    y = gated_rmsnorm(y.reshape(b, s, SSD_INNER), z, ssd_norm_w).astype(h.dtype)
    y_a = y @ w_ssd_out
    u = causal_depthwise_conv(sc_c * sc_h, sc_conv_w)
    y_b = (sc_b * u) @ w_sc_out
    merged = jax.nn.sigmoid(gate_a) * y_a + jax.nn.sigmoid(gate_b) * y_b
    return merged @ w_o


def moe_ffn(h, router_w, router_bias, w_gate, w_up, w_down, sh_gate, sh_up, sh_down):
    b, s, d = h.shape
    T = b * s
    hf = h.reshape(T, d)
    scores = jax.nn.sigmoid((hf @ router_w).astype(jnp.float32))
    biased = scores + router_bias.astype(jnp.float32)
    grp = biased.reshape(T, N_EXPERT_GROUPS, N_EXPERTS // N_EXPERT_GROUPS)
    grp_score = lax.top_k(grp, 2)[0].sum(-1)
    _, grp_idx = lax.top_k(grp_score, TOPK_EXPERT_GROUPS)
    grp_mask = jax.nn.one_hot(grp_idx, N_EXPERT_GROUPS, dtype=jnp.float32).sum(1) > 0
    expert_mask = jnp.repeat(grp_mask, N_EXPERTS // N_EXPERT_GROUPS, axis=1)
    _, e_idx = lax.top_k(jnp.where(expert_mask, biased, -jnp.inf), TOP_K)
    gate = jnp.take_along_axis(scores, e_idx, axis=1)
    gate = gate / jnp.sum(gate, axis=-1, keepdims=True) * ROUTED_SCALE
    n_assign = T * TOP_K
    nb = (n_assign + MOE_BLOCK - 1) // MOE_BLOCK + N_EXPERTS
    n_rows = nb * MOE_BLOCK
    e_flat = e_idx.reshape(-1)
    tok_flat = jnp.arange(n_assign, dtype=jnp.int32) // TOP_K
    order = jnp.argsort(e_flat)
    e_sorted = e_flat[order]
    tok_sorted = tok_flat[order]
    gate_sorted = gate.reshape(-1)[order]
    counts = jnp.bincount(e_flat, length=N_EXPERTS)
    padded = (counts + MOE_BLOCK - 1) // MOE_BLOCK * MOE_BLOCK
    start = jnp.cumsum(counts) - counts
    pend = jnp.cumsum(padded)
    pstart = pend - padded
    dest = pstart[e_sorted] + (jnp.arange(n_assign) - start[e_sorted])
    row_tok = jnp.full((n_rows,), T, jnp.int32).at[dest].set(tok_sorted)
    row_gate = jnp.zeros((n_rows,), jnp.float32).at[dest].set(gate_sorted)
    block_expert = jnp.minimum(
        jnp.searchsorted(pend, jnp.arange(nb) * MOE_BLOCK, side="right"), N_EXPERTS - 1)
    h_pad = jnp.concatenate([hf, jnp.zeros((1, d), hf.dtype)], axis=0)
    xin = h_pad[row_tok].reshape(nb, MOE_BLOCK, d)

    def expert_block(args):
        xb, e = args
        return (jax.nn.silu(xb @ w_gate[e]) * (xb @ w_up[e])) @ w_down[e]

    yb = lax.map(expert_block, (xin, block_expert)).reshape(n_rows, d)
    routed = jax.ops.segment_sum(yb * row_gate[:, None], row_tok, num_segments=T + 1)[:T]
    shared = (jax.nn.silu(hf @ sh_gate) * (hf @ sh_up)) @ sh_down
    return (routed + shared).reshape(b, s, d)


def setup_inputs(seed: int = 0) -> dict:
    key = jax.random.key(seed)
    ks = jax.random.split(key, 32)
    L, D, E = DEPTH, D_MODEL, N_EXPERTS
    f32 = jnp.float32

    def nrm(k, shape, fan_in, scale=1.0):
        return jax.random.normal(k, shape, f32) * (scale * fan_in ** -0.5)

    u = jax.random.uniform(ks[5], (L, SSD_HEADS), f32)
    dt0 = jnp.exp(u * (np.log(0.1) - np.log(1e-3)) + np.log(1e-3))
    return {
        "x": jax.random.normal(ks[0], (BATCH, SEQ, D), f32),
        "c": jax.random.normal(ks[1], (BATCH, D), f32),
        "w_ada": nrm(ks[2], (L, D, 6 * D), D),
        "b_ada": 0.02 * jax.random.normal(ks[3], (L, 6 * D), f32),
        "w_in": nrm(ks[4], (L, D, IN_COLS), D),
        "ssd_conv_w": nrm(ks[6], (L, SSD_CONV, SSD_CONV_DIM), SSD_CONV),
        "ssd_conv_b": 0.02 * jax.random.normal(ks[7], (L, SSD_CONV_DIM), f32),
        "ssd_dt_bias": dt0 + jnp.log(-jnp.expm1(-dt0)),
        "ssd_A_log": jnp.log(jax.random.uniform(ks[8], (L, SSD_HEADS), f32, 1.0, 16.0)),
        "ssd_D": 1.0 + 0.1 * jax.random.normal(ks[9], (L, SSD_HEADS), f32),
        "ssd_norm_w": 1.0 + 0.1 * jax.random.normal(ks[10], (L, SSD_INNER), f32),
        "w_ssd_out": nrm(ks[11], (L, SSD_INNER, D), SSD_INNER, BETA),
        "sc_conv_w": nrm(ks[12], (L, SC_KERNEL, SC_WIDTH), SC_KERNEL),
        "w_sc_out": nrm(ks[13], (L, SC_WIDTH, D), SC_WIDTH, BETA),
        "w_o": nrm(ks[14], (L, D, D), D, BETA),
        "ln1_g": 1.0 + 0.1 * jax.random.normal(ks[15], (L, D), f32),
        "ln1_b": 0.02 * jax.random.normal(ks[16], (L, D), f32),
        "router_w": nrm(ks[17], (L, D, E), D),
        "router_bias": 0.01 * jax.random.normal(ks[18], (L, E), f32),
        "w_gate": nrm(ks[19], (L, E, D, D_EXPERT), D, BETA),
        "w_up": nrm(ks[20], (L, E, D, D_EXPERT), D, BETA),
        "w_down": nrm(ks[21], (L, E, D_EXPERT, D), D_EXPERT, BETA),
        "sh_gate": nrm(ks[22], (L, D, D_EXPERT), D, BETA),
        "sh_up": nrm(ks[23], (L, D, D_EXPERT), D, BETA),
        "sh_down": nrm(ks[24], (L, D_EXPERT, D), D_EXPERT, BETA),
        "ln2_g": 1.0 + 0.1 * jax.random.normal(ks[25], (L, D), f32),
        "ln2_b": 0.02 * jax.random.normal(ks[26], (L, D), f32),
    }


def reference(x, c, w_ada, b_ada, w_in, ssd_conv_w, ssd_conv_b, ssd_dt_bias, ssd_A_log, ssd_D,
              ssd_norm_w, w_ssd_out, sc_conv_w, w_sc_out, w_o, ln1_g, ln1_b, router_w,
              router_bias, w_gate, w_up, w_down, sh_gate, sh_up, sh_down, ln2_g, ln2_b):
    dtype = x.dtype
    for l in range(DEPTH):
        mod = jax.nn.silu(c) @ w_ada[l] + b_ada[l]
        sh1, sc1, g1, sh2, sc2, g2 = jnp.split(mod[:, None, :], 6, axis=-1)
        h = (layer_norm(x) * (1.0 + sc1) + sh1).astype(dtype)
        mix = hybrid_mixer(h, w_in[l], ssd_conv_w[l], ssd_conv_b[l], ssd_dt_bias[l], ssd_A_log[l],
                           ssd_D[l], ssd_norm_w[l], w_ssd_out[l], sc_conv_w[l], w_sc_out[l], w_o[l])
        x = (layer_norm(ALPHA * x + g1 * mix) * ln1_g[l] + ln1_b[l]).astype(dtype)
        h = (layer_norm(x) * (1.0 + sc2) + sh2).astype(dtype)
        ffn = moe_ffn(h, router_w[l], router_bias[l], w_gate[l], w_up[l], w_down[l],
                      sh_gate[l], sh_up[l], sh_down[l])
        x = (layer_norm(ALPHA * x + g2 * ffn) * ln2_g[l] + ln2_b[l]).astype(dtype)
    return x
```

```python
import numpy as np
from contextlib import ExitStack
import concourse.bass as bass
import concourse.mybir as mybir
from concourse.bass_utils import run_bass_kernel_spmd

F32 = mybir.dt.float32
BF16 = mybir.dt.bfloat16
AF = mybir.ActivationFunctionType
ALU = mybir.AluOpType
AX = mybir.AxisListType

S = 4096
D = 1024
TT = 512
NTILE = S // TT
NE = 65
ALPHA = 2.0 ** 0.25
LN_EPS = 1e-5
RMS_EPS = 1e-5
INCOLS = 11296
C_Z, C_XBC, C_DT, C_SCB, C_SCC, C_SCH, C_GA, C_GB = 0, 2048, 6144, 6176, 7200, 8224, 9248, 10272
R_DTB, R_ALOG, R_D, R_RB, R_NW, R_L1G, R_L1B, R_L2G, R_L2B = 0, 32, 64, 96, 160, 2208, 3232, 4256, 5280
NROW = 6304
CV_CW, CV_CB, CV_SCW = 0, 128, 160
NCOL = 184
K_ID, K_TRIU, K_STRIL, K_ONES, K_EPSLN, K_EPSRMS, K_ONE = 0, 128, 256, 384, 512, 513, 514
NCONST = 516
ARENA_WORDS = 52600


class Buf:
    __slots__ = ("name", "w", "r")

    def __init__(self, name):
        self.name = name
        self.w = None
        self.r = {}


class Trk:
    ENGS = ("pe", "act", "dve", "pool", "sp")

    def __init__(self, nc, stack):
        self.nc = nc
        self.stack = stack
        self.sem = {}
        self.cnt = {}
        self.q = {e: [] for e in self.ENGS}
        self.seen = {e: {} for e in self.ENGS}
        for e in self.ENGS:
            self.newsem("E_" + e)

    def newsem(self, name):
        self.sem[name] = self.stack.enter_context(self.nc.semaphore(name))
        self.cnt[name] = 0
        return name

    def _waits(self, eng, reads, writes):
        need = {}
        own = "E_" + eng

        def add(s, v):
            if v > need.get(s, 0):
                need[s] = v
        for b in reads:
            if b.w:
                add(*b.w)
        for b in writes:
            if b.w:
                add(*b.w)
            for s, v in b.r.items():
                if s != own:
                    add(s, v)
        out = []
        for s, v in need.items():
            if s == own and eng == "pe":
                continue
            if self.seen[eng].get(s, 0) >= v:
                continue
            self.seen[eng][s] = v
            out.append((s, v))
        return out

    def op(self, eng, fn, reads=(), writes=()):
        w = self._waits(eng, reads, writes)
        s = "E_" + eng
        self.cnt[s] += 1
        v = self.cnt[s]
        self.q[eng].append((w, fn, s, 1))
        for b in reads:
            b.r[s] = v
        for b in writes:
            b.w = (s, v)
            b.r = {}

    def dma(self, eng, fns, dsem, reads=(), writes=()):
        w = self._waits(eng, reads, writes)
        for i, fn in enumerate(fns):
            self.q[eng].append((w if i == 0 else [], fn, dsem, 16))
        self.cnt[dsem] += 16 * len(fns)
        v = self.cnt[dsem]
        for b in reads:
            b.r[dsem] = v
        for b in writes:
            b.w = (dsem, v)
            b.r = {}

    def barrier(self, final=False):
        snap = dict(self.cnt)
        for e in self.ENGS:
            if e == "pool" and not final:
                continue
            w = []
            for s, v in snap.items():
                if v > 0 and self.seen[e].get(s, 0) < v and not (s == "E_" + e and e == "pe"):
                    self.seen[e][s] = v
                    w.append((s, v))
            if w:
                self.q[e].append((w, None, None, 0))

    def emit(self, block):
        sem = self.sem

        def run(h, q):
            for waits, fn, s, inc in q:
                for ws, wv in waits:
                    h.wait_ge(sem[ws], wv)
                if fn is not None:
                    fn(h).then_inc(sem[s], inc)

        @block.tensor
        def _(h):
            run(h, self.q["pe"])

        @block.scalar
        def _(h):
            run(h, self.q["act"])

        @block.vector
        def _(h):
            run(h, self.q["dve"])

        @block.gpsimd
        def _(h):
            run(h, self.q["pool"])

        @block.sync
        def _(h):
            run(h, self.q["sp"])


class Arena:
    def __init__(self, ap, nwords):
        self.ap = ap
        self.n = nwords
        self.top = 0

    def alloc(self, shape, dtype=F32):
        n = int(np.prod(shape))
        words = n if dtype == F32 else (n + 1) // 2
        off = self.top
        self.top += words
        assert self.top <= self.n, ("arena overflow", self.top, self.n)
        v = self.ap[:, off:off + words]
        if dtype == BF16:
            v = v.bitcast(BF16)
        if len(shape) == 2:
            v = v.rearrange("p (a b) -> p a b", a=shape[0])
        elif len(shape) == 3:
            v = v.rearrange("p (a b c) -> p a b c", a=shape[0], b=shape[1])
        return v


def build(cfg=None):
    cfg = cfg or {}
    n_tiles = cfg.get("n_tiles", NTILE)
    do_moe = cfg.get("moe", True)
    n_exp = cfg.get("n_exp", NE)
    taps = cfg.get("taps", ())
    nc = bass.Bass("TRN2", target_bir_lowering=False)

    def din(name, shape, dt=F32):
        return nc.dram_tensor(name, list(shape), dt, kind="ExternalInput").ap()

    x_d = din("x", [S, D])
    cT_d = din("cT", [128, 8])
    w_ada_d = din("w_ada", [D, 6 * D])
    b_ada_d = din("b_ada", [1, 6 * D])
    w_in_d = din("w_in", [D, INCOLS])
    colv_d = din("colv", [128, NCOL])
    rowv_d = din("rowv", [1, NROW])
    cst_d = din("consts", [128, NCONST])
    w_sso_d = din("w_ssd_out", [2048, D])
    w_sco_d = din("w_sc_out", [D, D])
    w_o_d = din("w_o", [D, D])
    rw_d = din("router_w", [D, 64])
    wg_d = din("w_gate", [NE, D, 256])
    wu_d = din("w_up", [NE, D, 256])
    wd_d = din("w_down", [NE, 256, D])
    out_d = nc.dram_tensor("out", [S, D], F32, kind="ExternalOutput").ap()
    x1_d = nc.dram_tensor("x1_scr", [S, D], F32, kind="Internal").ap()
    h2T_d = nc.dram_tensor("h2T_scr", [128, 8, S], BF16, kind="Internal").ap()

    tap_out = {}
    with ExitStack() as stack:
        arena_t = stack.enter_context(nc.sbuf_tensor("arena", [128, ARENA_WORDS], F32))
        ps_t = [stack.enter_context(nc.psum_tensor(f"ps{i}", [128, 512], F32)) for i in range(8)]
        T = Trk(nc, stack)
        AR = Arena(arena_t[:], ARENA_WORDS)
        PSB = [Buf(f"ps{i}") for i in range(8)]
        ps_rr = [0]

        def next_ps():
            i = ps_rr[0] % 8
            ps_rr[0] += 1
            return ps_t[i][:], PSB[i]

        dsem_n = [0]

        def new_dsem(tag):
            dsem_n[0] += 1
            return T.newsem(f"D{dsem_n[0]}_{tag}")

        tap_sem = new_dsem("tap")

        def tap(name, ap, buf):
            if name not in taps:
                return
            dt = ap.dtype
            t = nc.dram_tensor("tap_" + name, list(ap.shape), dt, kind="ExternalOutput").ap()
            tap_out[name] = t
            T.dma("sp", [lambda h, t=t, ap=ap: h.dma_start(out=t, in_=ap)], tap_sem, reads=[buf], writes=[])

        cst = AR.alloc([NCONST])
        identF = cst[:, K_ID:K_ID + 128]
        triU = cst[:, K_TRIU:K_TRIU + 128]
        striL = cst[:, K_STRIL:K_STRIL + 128]
        onesF = cst[:, K_ONES:K_ONES + 128]
        epsln = cst[:, K_EPSLN:K_EPSLN + 1]
        epsrms = cst[:, K_EPSRMS:K_EPSRMS + 1]
        onecol = cst[:, K_ONE:K_ONE + 1]
        identB = AR.alloc([128], BF16)
        rowv = AR.alloc([NROW])
        colv = AR.alloc([NCOL])
        modcol = AR.alloc([32])
        g1b = AR.alloc([D])
        g2b = AR.alloc([D])
        negA = AR.alloc([32])
        G_all = AR.alloc([32, NE])
        rw = AR.alloc([8, 64], BF16)
        halo = AR.alloc([32, 3])
        halo2 = AR.alloc([8, 2])
        state = AR.alloc([32, 64])
        stbf = AR.alloc([32, 64], BF16)
        B_cst, B_identB, B_rowv, B_colv, B_modcol, B_g1b, B_g2b, B_negA, B_G, B_rw = (
            Buf(n) for n in ("cst", "identB", "rowv", "colv", "modcol", "g1b", "g2b", "negA", "G", "rw"))
        B_halo = [Buf(f"halo{i}") for i in range(32)]
        B_halo2 = [Buf(f"halo2_{i}") for i in range(8)]
        B_state = [Buf(f"state{g}") for g in range(8)]
        B_stbf = [Buf(f"stbf{g}") for g in range(8)]
        PERSIST_TOP = AR.top

        ld_sem = new_dsem("ld")
        T.dma("sp", [lambda h: h.dma_start(out=cst, in_=cst_d[:, :])], new_dsem("cst"), writes=[B_cst])
        T.dma("sp", [lambda h: h.dma_start(out=rowv, in_=rowv_d[0, :].partition_broadcast(128))], new_dsem("rowv"), writes=[B_rowv])
        T.dma("sp", [lambda h: h.dma_start(out=colv, in_=colv_d[:, :])], new_dsem("colv"), writes=[B_colv])
        rw_sem = new_dsem("rw")
        T.dma("pool", [lambda h: h.dma_start(out=rw, in_=rw_d.rearrange("(kc p) n -> p kc n", p=128))], rw_sem, writes=[B_rw])
        T.op("dve", lambda h: h.tensor_copy(out=identB, in_=identF), reads=[B_cst], writes=[B_identB])
        T.op("act", lambda h: h.activation(out=negA, in_=rowv[:, R_ALOG:R_ALOG + 32], func=AF.Exp), reads=[B_rowv], writes=[B_negA])
        T.op("dve", lambda h: h.tensor_scalar(out=negA, in0=negA, scalar1=-1.0, scalar2=None, op0=ALU.mult), reads=[B_negA], writes=[B_negA])
        T.op("dve", lambda h: h.memset(halo, 0.0), writes=B_halo)
        T.op("dve", lambda h: h.memset(halo2, 0.0), writes=B_halo2)
        T.op("dve", lambda h: h.memset(state, 0.0), writes=B_state)
        T.op("dve", lambda h: h.memset(stbf, 0.0), writes=B_stbf)
        T.op("dve", lambda h: h.memset(G_all, 1.0), writes=[B_G])

        cTt = AR.alloc([8])
        scT = AR.alloc([8])
        modrow = AR.alloc([6 * D])
        badar = AR.alloc([6 * D])
        wa = [AR.alloc([8, 512]) for _ in range(2)]
        B_cT, B_scT, B_modrow, B_badar = Buf("cT"), Buf("scT"), Buf("modrow"), Buf("badar")
        B_wa = [Buf("wa0"), Buf("wa1")]
        wa_sem = [new_dsem("wa0"), new_dsem("wa1")]
        T.dma("sp", [lambda h: h.dma_start(out=cTt, in_=cT_d[:, :])], new_dsem("cT"), writes=[B_cT])
        T.dma("sp", [lambda h: h.dma_start(out=badar[0:1, :], in_=b_ada_d[:, :])], new_dsem("bada"), writes=[B_badar])
        T.op("act", lambda h: h.activation(out=scT, in_=cTt, func=AF.Silu), reads=[B_cT], writes=[B_scT])
        for nb in range(12):
            sl = nb % 2
            T.dma("sp", [lambda h, nb=nb, sl=sl: h.dma_start(
                out=wa[sl], in_=w_ada_d[:, nb * 512:(nb + 1) * 512].rearrange("(kc p) n -> p kc n", p=128))],
                wa_sem[sl], writes=[B_wa[sl]])
            ps, pb = next_ps()

            def mm(h, ps=ps, sl=sl):
                for kc in range(8):
                    r = h.matmul(ps[0:1, :], scT[:, kc:kc + 1], wa[sl][:, kc, :], start=(kc == 0), stop=(kc == 7))
                return r
            T.op("pe", mm, reads=[B_scT, B_wa[sl]], writes=[pb])
            T.op("dve", lambda h, ps=ps, nb=nb: h.tensor_tensor(
                out=modrow[0:1, nb * 512:(nb + 1) * 512], in0=ps[0:1, :], in1=badar[0:1, nb * 512:(nb + 1) * 512], op=ALU.add),
                reads=[pb, B_badar], writes=[B_modrow])
        ps, pb = next_ps()

        def mmcol(h, ps=ps):
            for j in range(32):
                off = [0, 1024, 3072, 4096][j // 8] + (j % 8) * 128
                r = h.matmul(ps[:, j:j + 1], modrow[0:1, off:off + 128], onecol[0:1, 0:1], start=True, stop=True)
            return r
        T.op("pe", mmcol, reads=[B_modrow, B_cst], writes=[pb])
        T.op("dve", lambda h, ps=ps: h.tensor_copy(out=modcol, in_=ps[:, 0:32]), reads=[pb], writes=[B_modcol])
        T.op("dve", lambda h: h.tensor_scalar(out=modcol[:, 8:16], in0=modcol[:, 8:16], scalar1=1.0, scalar2=None, op0=ALU.add),
             reads=[B_modcol], writes=[B_modcol])
        T.op("dve", lambda h: h.tensor_scalar(out=modcol[:, 24:32], in0=modcol[:, 24:32], scalar1=1.0, scalar2=None, op0=ALU.add),
             reads=[B_modcol], writes=[B_modcol])
        for gi, (gb_, bb) in enumerate(((g1b, B_g1b), (g2b, B_g2b))):
            for hf in range(2):
                ps, pb = next_ps()
                off = (2048 if gi == 0 else 5120) + hf * 512
                T.op("pe", lambda h, ps=ps, off=off: h.matmul(ps, onesF[0:1, :], modrow[0:1, off:off + 512], start=True, stop=True),
                     reads=[B_modrow, B_cst], writes=[pb])
                T.op("act", lambda h, ps=ps, gb_=gb_, hf=hf: h.copy(out=gb_[:, hf * 512:(hf + 1) * 512], in_=ps),
                     reads=[pb], writes=[bb])
        tap("modrow", modrow[0:1, :], B_modrow)
        tap("modcol", modcol, B_modcol)
        tap("g1b", g1b, B_g1b)
        T.barrier(final=True)
        AR.top = PERSIST_TOP

        def ln_stats_g(src, B_src, st, mv, rstd, B_small):
            T.op("dve", lambda h: h.bn_stats(out=st[:, 0, :], in_=src[:, 0:512]), reads=[B_src], writes=[B_small])
            yield
            T.op("dve", lambda h: h.bn_stats(out=st[:, 1, :], in_=src[:, 512:1024]), reads=[B_src], writes=[B_small])
            yield
            T.op("dve", lambda h: h.bn_aggr(out=mv[:, 0:2], in_=st.rearrange("p a b -> p (a b)")), reads=[B_small], writes=[B_small])
            yield
            T.op("act", lambda h: h.activation(out=mv[:, 2:3], in_=mv[:, 1:2], func=AF.Sqrt, bias=epsln, scale=1.0),
                 reads=[B_small, B_cst], writes=[B_small])
            yield
            T.op("dve", lambda h: h.reciprocal(out=rstd, in_=mv[:, 2:3]), reads=[B_small], writes=[B_small])
            yield

        def ln_stats(*a):
            for _ in ln_stats_g(*a):
                pass

        def ln_mod_T_g(src, B_src, xn, B_xn, st, mv, B_small, colbase, dst_fn, B_dst):
            rstd = mv[:, 3:4]
            yield from ln_stats_g(src, B_src, st, mv, rstd, B_small)
            T.op("dve", lambda h: h.tensor_scalar(out=xn, in0=src, scalar1=mv[:, 0:1], scalar2=rstd, op0=ALU.subtract, op1=ALU.mult),
                 reads=[B_src, B_small], writes=[B_xn])
            yield
            ps, pb = next_ps()
            psb = ps.bitcast(BF16)

            def tr(h):
                for kc in range(8):
                    r = h.transpose(psb[:, kc * 128:(kc + 1) * 128], xn[:, kc * 128:(kc + 1) * 128], identB)
                return r
            T.op("pe", tr, reads=[B_xn, B_identB], writes=[pb])
            yield
            for kc in range(8):
                T.op("act", lambda h, kc=kc: h.activation(
                    out=dst_fn(kc), in_=psb[:, kc * 128:(kc + 1) * 128], func=AF.Identity,
                    scale=modcol[:, colbase + 8 + kc:colbase + 9 + kc], bias=modcol[:, colbase + kc:colbase + kc + 1]),
                    reads=[pb, B_modcol], writes=[B_dst])
                yield

        def ln_mod_T(*a):
            for _ in ln_mod_T_g(*a):
                pass

        def run_interleaved(gens):
            gens = [g for g in gens if g is not None]
            while gens:
                for g in list(gens):
                    try:
                        next(g)
                    except StopIteration:
                        gens.remove(g)

        hT = AR.alloc([8, TT], BF16)
        sz = AR.alloc([4, 2048], BF16)
        xbcT = AR.alloc([32, TT], BF16)
        xs_tok = AR.alloc([4, 2048], BF16)
        B_tok = AR.alloc([4, 1024], BF16)
        mergedT = B_tok.rearrange("p a b -> p (a b)").rearrange("p (a b) -> p a b", a=8)
        ubT = AR.alloc([8, TT], BF16)
        dtt = AR.alloc([4, 32])
        wblk = [AR.alloc([8, 512], BF16) for _ in range(2)]
        B_sz, B_xsT, B_BCT, B_ubT, B_dt = (
            Buf(n) for n in ("sz", "xsT", "BCT", "ubT", "dt"))
        B_hTs = [Buf(f"hT{i}") for i in range(4)]
        B_xstoks = [Buf(f"xstok{i}") for i in range(4)]
        B_Btoks = [Buf(f"Btok{i}") for i in range(4)]
        B_wblk = [Buf("wblk0"), Buf("wblk1")]
        wblk_sem = [new_dsem("wblk0"), new_dsem("wblk1")]
        wrr = [0]
        x_sem = [new_dsem("x0"), new_dsem("x1")]
        x1st_sem = [new_dsem("x1st0"), new_dsem("x1st1")]
        h2st_sem = [new_dsem("h2st0"), new_dsem("h2st1")]
        UNION_TOP = AR.top

        def load_w(src_ap, shape3):
            sl = wrr[0] % 2
            wrr[0] += 1
            flat = wblk[sl].rearrange("p a b -> p (a b)")
            n = shape3[0] * shape3[1]
            v = flat[:, 0:n].rearrange("p (a b) -> p a b", a=shape3[0])
            T.dma("pool", [lambda h, v=v, src_ap=src_ap: h.dma_start(out=v, in_=src_ap)], wblk_sem[sl], writes=[B_wblk[sl]])
            return v, B_wblk[sl]

        def win_cols(c0, n):
            return w_in_d[:, c0:c0 + n].rearrange("(kc p) n -> p kc n", p=128)

        def proj_fm(wv, wb, j, ps, pb):
            def mm(h):
                for kc in range(8):
                    r = h.matmul(ps, wv[:, kc, j * 128:(j + 1) * 128], hT[:, kc, :], start=(kc == 0), stop=(kc == 7))
                return r
            T.op("pe", mm, reads=[wb] + B_hTs, writes=[pb])

        for t in range(n_tiles):
            def stage_P(t=t):
                AR.top = UNION_TOP
                xt = [AR.alloc([D]) for _ in range(2)]
                xn = AR.alloc([D], BF16)
                st6 = AR.alloc([2, 6])
                mv = AR.alloc([8])
                pre = [AR.alloc([TT + 3]) for _ in range(2)]
                preb = [AR.alloc([TT + 4], BF16) for _ in range(2)]
                diag = [AR.alloc([4, 128], BF16) for _ in range(2)]
                sccbuf = AR.alloc([4, TT])
                dtmp = AR.alloc([2, 32])
                B_xt = [Buf("xt0"), Buf("xt1")]
                B_xn, B_small = Buf("xn"), Buf("lnsmall")
                B_pre = [Buf("pre0"), Buf("pre1")]
                B_preb = [Buf("preb0"), Buf("preb1")]
                B_diag = [Buf("diag0"), Buf("diag1")]
                B_scc = [Buf(f"scc{j}") for j in range(4)]
                B_dtmp = Buf("dtmp")
                for ci in range(4):
                    tok0 = t * TT + ci * 128
                    sl = ci % 2
                    T.dma("sp", [lambda h, sl=sl, tok0=tok0: h.dma_start(out=xt[sl], in_=x_d[tok0:tok0 + 128, :])], x_sem[sl], writes=[B_xt[sl]])
                    ln_mod_T(xt[sl], B_xt[sl], xn, B_xn, st6, mv, B_small, 0,
                             lambda kc, ci=ci: hT[:, kc, ci * 128:(ci + 1) * 128], B_hTs[ci])
                    if t == 0 and ci == 0:
                        tap("xt0", xt[sl], B_xt[sl])
                        tap("xn0", xn, B_xn)
                        tap("mv0", mv, B_small)
                        if cfg.get("stop") == "ln0":
                            tap("hT", hT, B_hTs[3])
                            return 'stop'
                if t == 0:
                    tap("hT", hT, B_hTs[3])
                for blk in range(4):
                    wv, wb = load_w(win_cols(C_Z + blk * 512, 512), (8, 512))
                    for ci in range(4):
                        ps, pb = next_ps()

                        def mm(h, ps=ps, wv=wv, ci=ci):
                            for kc in range(8):
                                r = h.matmul(ps, hT[:, kc, ci * 128:(ci + 1) * 128], wv[:, kc, :], start=(kc == 0), stop=(kc == 7))
                            return r
                        T.op("pe", mm, reads=[wb, B_hTs[ci]], writes=[pb])
                        T.op("act", lambda h, ps=ps, ci=ci, blk=blk: h.activation(out=sz[:, ci, blk * 512:(blk + 1) * 512], in_=ps, func=AF.Silu),
                             reads=[pb], writes=[B_sz])
                wcur = [None]

                def xbc_front(idx):
                    blk, j = idx // 4, idx % 4
                    if j == 0:
                        wcur[0] = load_w(win_cols(C_XBC + blk * 512, 512), (8, 512))
                    wv, wb = wcur[0]
                    ch = idx
                    sl = idx % 2
                    ps, pb = next_ps()
                    proj_fm(wv, wb, j, ps, pb)
                    T.op("act", lambda h: h.copy(out=preb[sl][:, 3:TT + 3], in_=ps), reads=[pb], writes=[B_preb[sl]])
                    T.op("dve", lambda h: h.tensor_copy(out=preb[sl][:, 0:3], in_=halo[:, ch, :]), reads=[B_halo[ch]], writes=[B_preb[sl]])
                    T.op("dve", lambda h: h.tensor_copy(out=halo[:, ch, :], in_=preb[sl][:, TT:TT + 3]), reads=[B_preb[sl]], writes=[B_halo[ch]])
                    cw0 = CV_CW + ch * 4
                    for k in range(4):
                        T.op("dve", lambda h, k=k: h.tensor_scalar(
                            out=diag[sl][:, k, :], in0=identB, scalar1=colv[:, cw0 + k:cw0 + k + 1], scalar2=None, op0=ALU.mult),
                            reads=[B_identB, B_colv], writes=[B_diag[sl]])

                def xbc_back(idx):
                    ch = idx
                    sl = idx % 2
                    ps2, pb2 = next_ps()

                    def mmc(h):
                        for k in range(4):
                            r = h.matmul(ps2, diag[sl][:, k, :], preb[sl][:, k:k + TT], start=(k == 0), stop=(k == 3))
                        return r
                    T.op("pe", mmc, reads=[B_diag[sl], B_preb[sl]], writes=[pb2])
                    T.op("act", lambda h: h.activation(
                        out=xbcT[:, ch, :], in_=ps2, func=AF.Silu, bias=colv[:, CV_CB + ch:CV_CB + ch + 1], scale=1.0),
                        reads=[pb2, B_colv], writes=[B_xsT if ch < 16 else B_BCT])

                xbc_front(0)
                for idx in range(32):
                    if idx + 1 < 32:
                        xbc_front(idx + 1)
                    xbc_back(idx)
                wv, wb = load_w(win_cols(C_DT, 32), (8, 32))
                for ci in range(4):
                    ps, pb = next_ps()

                    def mm(h, ps=ps, wv=wv, ci=ci):
                        for kc in range(8):
                            r = h.matmul(ps[:, 0:32], hT[:, kc, ci * 128:(ci + 1) * 128], wv[:, kc, :], start=(kc == 0), stop=(kc == 7))
                        return r
                    T.op("pe", mm, reads=[wb, B_hTs[ci]], writes=[pb])
                    T.op("dve", lambda h, ps=ps: h.tensor_tensor(out=dtmp[:, 0, :], in0=ps[:, 0:32], in1=rowv[:, R_DTB:R_DTB + 32], op=ALU.add),
                         reads=[pb, B_rowv], writes=[B_dtmp])
                    T.op("act", lambda h: h.activation(out=dtmp[:, 1, :], in_=dtmp[:, 0, :], func=AF.Exp), reads=[B_dtmp], writes=[B_dtmp])
                    T.op("act", lambda h, ci=ci: h.activation(out=dtt[:, ci, :], in_=dtmp[:, 1, :], func=AF.Ln, bias=onecol, scale=1.0),
                         reads=[B_dtmp, B_cst], writes=[B_dt])
                for k2 in range(2):
                    wv, wb = load_w(win_cols(C_SCC + k2 * 512, 512), (8, 512))
                    for j in range(4):
                        ps, pb = next_ps()
                        proj_fm(wv, wb, j, ps, pb)
                        T.op("act", lambda h, ps=ps, j=j: h.copy(out=sccbuf[:, j, :], in_=ps), reads=[pb], writes=[B_scc[j]])
                    wv, wb = load_w(win_cols(C_SCH + k2 * 512, 512), (8, 512))
                    for j in range(4):
                        ch8 = k2 * 4 + j
                        ps, pb = next_ps()
                        proj_fm(wv, wb, j, ps, pb)
                        sl = j % 2
                        T.op("dve", lambda h, ps=ps, j=j, sl=sl: h.tensor_tensor(out=pre[sl][:, 2:TT + 2], in0=ps, in1=sccbuf[:, j, :], op=ALU.mult),
                             reads=[pb, B_scc[j]], writes=[B_pre[sl]])
                        T.op("dve", lambda h, sl=sl, ch8=ch8: h.tensor_copy(out=pre[sl][:, 0:2], in_=halo2[:, ch8, :]), reads=[B_halo2[ch8]], writes=[B_pre[sl]])
                        T.op("dve", lambda h, sl=sl, ch8=ch8: h.tensor_copy(out=halo2[:, ch8, :], in_=pre[sl][:, TT:TT + 2]), reads=[B_pre[sl]], writes=[B_halo2[ch8]])
                        c0 = CV_SCW + ch8 * 3
                        T.op("dve", lambda h, sl=sl, j=j, c0=c0: h.tensor_scalar(
                            out=sccbuf[:, j, :], in0=pre[sl][:, 0:TT], scalar1=colv[:, c0:c0 + 1], scalar2=None, op0=ALU.mult),
                            reads=[B_pre[sl], B_colv], writes=[B_scc[j]])
                        for k in range(1, 3):
                            T.op("dve", lambda h, sl=sl, j=j, k=k, c0=c0: h.scalar_tensor_tensor(
                                out=sccbuf[:, j, :], in0=pre[sl][:, k:k + TT], scalar=colv[:, c0 + k:c0 + k + 1], in1=sccbuf[:, j, :],
                                op0=ALU.mult, op1=ALU.add), reads=[B_pre[sl], B_colv, B_scc[j]], writes=[B_scc[j]])
                    wv, wb = load_w(win_cols(C_SCB + k2 * 512, 512), (8, 512))
                    for j in range(4):
                        ch8 = k2 * 4 + j
                        ps, pb = next_ps()
                        proj_fm(wv, wb, j, ps, pb)
                        T.op("dve", lambda h, ps=ps, j=j, ch8=ch8: h.tensor_tensor(out=ubT[:, ch8, :], in0=ps, in1=sccbuf[:, j, :], op=ALU.mult),
                             reads=[pb, B_scc[j]], writes=[B_ubT])
                if t == 0:
                    tap("hT_end", hT, B_hTs[3])
                    tap("sz", sz, B_sz)
                    tap("xsT", xbcT[:, 0:16, :], B_xsT)
                    tap("BCT", xbcT[:, 16:32, :], B_BCT)
                    tap("dt", dtt, B_dt)
                    tap("ubT", ubT, B_ubT)
                T.barrier()
                if cfg.get("stop") == "P":
                    return 'stop'
                return None

            if stage_P() == 'stop':
                break
            def stage_S(t=t):
                AR.top = UNION_TOP
                dA = AR.alloc([32])
                sm = AR.alloc([6, 32])
                triDA = [AR.alloc([4, 128]) for _ in range(3)]
                cbm = AR.alloc([8, 128])
                LT = [AR.alloc([4, 128]) for _ in range(2)]
                scTt = [AR.alloc([4, 128], BF16) for _ in range(2)]
                gtmp = AR.alloc([2048])
                Xb = gtmp[:, 0:1024].bitcast(BF16).rearrange("p (a b) -> p a b", a=32)
                Xdec = gtmp[:, 1024:2048].bitcast(BF16).rearrange("p (a b) -> p a b", a=32)
                ytok = AR.alloc([32, 64])
                ytmp = [AR.alloc([4, 64]) for _ in range(2)]
                gn_tok = AR.alloc([2048], BF16)
                rms = AR.alloc([24])
                B_dA, B_sm, B_cbm, B_X, B_ytok, B_gntok, B_rms = (
                    Buf(n) for n in ("dA", "sm", "cbm", "XG", "ytok", "gntok", "rms"))
                B_Xdec = B_X
                B_gtmp = B_X
                B_triDA = [Buf("triDA0"), Buf("triDA1"), Buf("triDA2")]
                B_LT = [Buf("LT0"), Buf("LT1")]
                B_scTt = [Buf("scT0"), Buf("scT1")]
                B_ytmp = [Buf("ytmp0"), Buf("ytmp1")]
                def trans(ci):
                    for grp8 in range(3):
                        ps, pb = next_ps()
                        psb = ps.bitcast(BF16)

                        def tr(h, psb=psb, grp8=grp8):
                            for q in range(8):
                                ch = grp8 * 8 + q
                                r = h.transpose(psb[:, q * 128:(q + 1) * 128], xbcT[:, ch, ci * 128:(ci + 1) * 128], identB)
                            return r
                        T.op("pe", tr, reads=[B_xsT if grp8 < 2 else B_BCT, B_identB], writes=[pb])
                        if grp8 < 2:
                            T.op("act", lambda h, psb=psb, grp8=grp8: h.copy(out=xs_tok[:, ci, grp8 * 1024:(grp8 + 1) * 1024], in_=psb),
                                 reads=[pb], writes=[B_xstoks[ci]])
                        else:
                            T.op("act", lambda h, psb=psb: h.copy(out=B_tok[:, ci, :], in_=psb), reads=[pb], writes=[B_Btoks[ci]])
                if cfg.get("stop") == "S_tr":
                    for ci in range(4):
                        trans(ci)
                else:
                    trans(0)
                if cfg.get("stop") == "S_tr":
                    tap("xstok", xs_tok, B_xstoks[3])
                    T.barrier()
                    return 'stop'
                gcount = 0
                for ci in range(4):
                    cs = slice(ci * 128, (ci + 1) * 128)
                    xs3 = xs_tok[:, ci, :].rearrange("p (a b) -> p a b", a=32)
                    T.op("dve", lambda h, ci=ci: h.tensor_tensor(out=dA, in0=dtt[:, ci, :], in1=negA, op=ALU.mult), reads=[B_dt, B_negA], writes=[B_dA])
                    ps, pb = next_ps()

                    def mmA(h, ps=ps):
                        h.matmul(ps[:, 0:32], triU, dA, start=True, stop=True)
                        return h.matmul(ps[:, 32:64], onesF, dA, start=True, stop=True)
                    T.op("pe", mmA, reads=[B_cst, B_dA], writes=[pb])
                    T.op("act", lambda h, ps=ps: h.copy(out=sm[:, 0, :], in_=ps[:, 0:32]), reads=[pb], writes=[B_sm])
                    T.op("act", lambda h, ps=ps: h.activation(out=sm[:, 1, :], in_=ps[:, 0:32], func=AF.Exp), reads=[pb], writes=[B_sm])
                    T.op("act", lambda h, ps=ps: h.activation(out=sm[:, 3, :], in_=ps[:, 32:64], func=AF.Exp), reads=[pb], writes=[B_sm])
                    T.op("dve", lambda h, ps=ps: h.tensor_tensor(out=sm[:, 2, :], in0=ps[:, 32:64], in1=sm[:, 0, :], op=ALU.subtract), reads=[pb, B_sm], writes=[B_sm])
                    T.op("act", lambda h: h.activation(out=sm[:, 2, :], in_=sm[:, 2, :], func=AF.Exp), reads=[B_sm], writes=[B_sm])
                    T.op("dve", lambda h, ci=ci: h.tensor_tensor(out=sm[:, 4, :], in0=sm[:, 2, :], in1=dtt[:, ci, :], op=ALU.mult), reads=[B_sm, B_dt], writes=[B_sm])
                    T.op("dve", lambda h, xs3=xs3, ci=ci: h.tensor_tensor(
                        out=Xb, in0=xs3, in1=dtt[:, ci, :].unsqueeze(2).to_broadcast([128, 32, 64]), op=ALU.mult), reads=[B_xstoks[ci], B_dt], writes=[B_X])
                    T.op("dve", lambda h, xs3=xs3: h.tensor_tensor(
                        out=Xdec, in0=xs3, in1=sm[:, 4, :].unsqueeze(2).to_broadcast([128, 32, 64]), op=ALU.mult), reads=[B_xstoks[ci], B_sm], writes=[B_Xdec])
                    if cfg.get("stop") == "S_pre":
                        tap("sm", sm, B_sm)
                        tap("Xb", Xb, B_X)
                        T.barrier()
                        return 'stop'
                    for half in range(2):
                        ps, pb = next_ps()

                        def mmcb(h, ps=ps, half=half, cs=cs):
                            for q in range(4):
                                g = half * 4 + q
                                r = h.matmul(ps[:, q * 128:(q + 1) * 128], xbcT[:, 16 + g, cs], xbcT[:, 24 + g, cs], start=True, stop=True)
                            return r
                        T.op("pe", mmcb, reads=[B_BCT], writes=[pb])
                        T.op("dve", lambda h, ps=ps, half=half: h.tensor_tensor(
                            out=cbm[:, half * 4:(half + 1) * 4, :], in0=ps.rearrange("p (a b) -> p a b", a=4),
                            in1=triU.unsqueeze(1).to_broadcast([128, 4, 128]), op=ALU.mult), reads=[pb, B_cst], writes=[B_cbm])
                    if cfg.get("stop") == "S_cb":
                        tap("cbm", cbm, B_cbm)
                        T.barrier()
                        return 'stop'
                    def front0(g):
                        s3 = g % 3
                        hs = slice(4 * g, 4 * g + 4)
                        T.op("pool", lambda h: h.tensor_tensor(
                            out=triDA[s3], in0=triU.unsqueeze(1).to_broadcast([128, 4, 128]),
                            in1=dA[:, hs].unsqueeze(2).to_broadcast([128, 4, 128]), op=ALU.mult), reads=[B_cst, B_dA], writes=[B_triDA[s3]])

                    def front1(g):
                        sl = g % 2
                        s3 = g % 3
                        ps_s, pb_s = next_ps()
                        T.op("pe", lambda h: h.matmul(ps_s, striL, triDA[s3].rearrange("p a b -> p (a b)"), start=True, stop=True),
                             reads=[B_cst, B_triDA[s3]], writes=[pb_s])
                        T.op("act", lambda h: h.activation(out=LT[sl].rearrange("p a b -> p (a b)"), in_=ps_s, func=AF.Exp),
                             reads=[pb_s], writes=[B_LT[sl]])

                    def front2(g):
                        sl = g % 2
                        T.op("dve", lambda h, sl=sl, g=g: h.tensor_tensor(
                            out=scTt[sl], in0=LT[sl], in1=cbm[:, g, :].unsqueeze(1).to_broadcast([128, 4, 128]), op=ALU.mult),
                            reads=[B_LT[sl], B_cbm], writes=[B_scTt[sl]])

                    def back(g, cs=cs, ci=ci):
                        sl = g % 2
                        hs = slice(4 * g, 4 * g + 4)
                        ps_y, pb_y = next_ps()

                        def mmy(h, ps_y=ps_y, g=g, sl=sl, hs=hs, cs=cs):
                            h.matmul(ps_y[:, 0:256], xbcT[:, 24 + g, cs], stbf[:, hs, :].rearrange("p a b -> p (a b)"), start=True, stop=True)
                            for hh in range(4):
                                r = h.matmul(ps_y[:, 256 + hh * 64:256 + (hh + 1) * 64], scTt[sl][:, hh, :], Xb[:, 4 * g + hh, :], start=True, stop=True)
                            return r
                        T.op("pe", mmy, reads=[B_BCT, B_stbf[g], B_scTt[sl], B_X], writes=[pb_y])
                        ps_n, pb_n = next_ps()
                        T.op("pe", lambda h, ps_n=ps_n, g=g, ci=ci, hs=hs: h.matmul(
                            ps_n[:, 0:256], B_tok[:, ci, g * 128:(g + 1) * 128], Xdec[:, hs, :].rearrange("p a b -> p (a b)"), start=True, stop=True),
                            reads=[B_Btoks[ci], B_Xdec], writes=[pb_n])
                        return sl, hs, ps_y, pb_y, ps_n, pb_n

                    def back2(g, sl, hs, ps_y, pb_y, ps_n, pb_n):
                        for hh in range(4):
                            T.op("act", lambda h, hh=hh: h.activation(
                                out=ytmp[sl][:, hh, :], in_=ps_y[:, hh * 64:(hh + 1) * 64], func=AF.Copy,
                                scale=sm[:, 1, 4 * g + hh:4 * g + hh + 1]), reads=[pb_y, B_sm], writes=[B_ytmp[sl]])
                        T.op("dve", lambda h: h.tensor_tensor(
                            out=ytok[:, hs, :], in0=ps_y[:, 256:512].rearrange("p (a b) -> p a b", a=4), in1=ytmp[sl], op=ALU.add),
                            reads=[pb_y, B_ytmp[sl]], writes=[B_ytok])
                        T.op("dve", lambda h: h.tensor_tensor(
                            out=state[:, hs, :], in0=state[:, hs, :], in1=sm[:, 3, hs].unsqueeze(2).to_broadcast([128, 4, 64]), op=ALU.mult),
                            reads=[B_state[g], B_sm], writes=[B_state[g]])
                        T.op("dve", lambda h: h.tensor_tensor(
                            out=state[:, hs, :], in0=state[:, hs, :], in1=ps_n[:, 0:256].rearrange("p (a b) -> p a b", a=4), op=ALU.add),
                            reads=[B_state[g], pb_n], writes=[B_state[g]])
                        T.op("pool", lambda h: h.tensor_copy(out=stbf[:, hs, :], in_=state[:, hs, :]), reads=[B_state[g]], writes=[B_stbf[g]])

                    front0(0)
                    front0(1)
                    front1(0)
                    front2(0)
                    for g in range(8):
                        if g + 2 < 8:
                            front0(g + 2)
                        if g + 1 < 8:
                            front1(g + 1)
                        bk = back(g)
                        if g + 1 < 8:
                            front2(g + 1)
                        back2(g, *bk)
                    if ci + 1 < 4:
                        trans(ci + 1)
                    if cfg.get("stop") == "S_grp":
                        tap("ytok", ytok, B_ytok)
                        T.barrier()
                        return 'stop'
                    g3 = gtmp.rearrange("p (a b) -> p a b", a=32)
                    T.op("dve", lambda h, xs3=xs3, g3=g3: h.tensor_tensor(
                        out=g3, in0=xs3, in1=rowv[:, R_D:R_D + 32].unsqueeze(2).to_broadcast([128, 32, 64]), op=ALU.mult),
                        reads=[B_xstoks[ci], B_rowv], writes=[B_gtmp])
                    T.op("dve", lambda h, g3=g3: h.tensor_tensor(out=ytok, in0=ytok, in1=g3, op=ALU.add), reads=[B_ytok, B_gtmp], writes=[B_ytok])
                    if t == 0 and ci == 1:
                        tap("y1", ytok, B_ytok)
                    T.op("dve", lambda h, ci=ci: h.tensor_tensor(out=gtmp, in0=ytok.rearrange("p a b -> p (a b)"), in1=sz[:, ci, :], op=ALU.mult),
                         reads=[B_ytok, B_sz], writes=[B_gtmp])
                    for g in range(8):
                        T.op("act", lambda h, g=g: h.activation(
                            out=ytok.rearrange("p a b -> p (a b)")[:, g * 256:(g + 1) * 256], in_=gtmp[:, g * 256:(g + 1) * 256],
                            func=AF.Square, accum_out=rms[:, g:g + 1]), reads=[B_gtmp], writes=[B_ytok, B_rms])
                    T.op("act", lambda h: h.activation(out=rms[:, 8:16], in_=rms[:, 0:8], func=AF.Sqrt, bias=epsrms, scale=1.0 / 256.0),
                         reads=[B_rms, B_cst], writes=[B_rms])
                    T.op("dve", lambda h: h.reciprocal(out=rms[:, 16:24], in_=rms[:, 8:16]), reads=[B_rms], writes=[B_rms])
                    for g in range(8):
                        T.op("dve", lambda h, g=g: h.scalar_tensor_tensor(
                            out=gn_tok[:, g * 256:(g + 1) * 256], in0=gtmp[:, g * 256:(g + 1) * 256], scalar=rms[:, 16 + g:17 + g],
                            in1=rowv[:, R_NW + g * 256:R_NW + (g + 1) * 256], op0=ALU.mult, op1=ALU.mult),
                            reads=[B_gtmp, B_rms, B_rowv], writes=[B_gntok])
                    if t == 0 and ci == 1:
                        tap("gn1", gn_tok, B_gntok)
                    if cfg.get("stop") == "S_gn":
                        tap("gntok", gn_tok, B_gntok)
                        T.barrier()
                        return 'stop'
                    for grp8 in range(2):
                        ps, pb = next_ps()
                        psb = ps.bitcast(BF16)

                        def tr2(h, psb=psb, grp8=grp8):
                            for q in range(8):
                                c0 = (grp8 * 8 + q) * 128
                                r = h.transpose(psb[:, q * 128:(q + 1) * 128], gn_tok[:, c0:c0 + 128], identB)
                            return r
                        T.op("pe", tr2, reads=[B_gntok, B_identB], writes=[pb])
                        T.op("act", lambda h, psb=psb, grp8=grp8, cs=cs: h.copy(
                            out=xbcT[:, grp8 * 8:(grp8 + 1) * 8, cs], in_=psb.rearrange("p (a b) -> p a b", a=8)),
                            reads=[pb], writes=[B_xsT])
                T.barrier()

            if stage_S() == 'stop':
                break
            def stage_O(t=t):
                AR.top = UNION_TOP
                gnT = xbcT
                sg = [AR.alloc([TT]) for _ in range(2)]
                m1 = [AR.alloc([TT]) for _ in range(4)]
                u = [AR.alloc([D]) for _ in range(2)]
                xt = [AR.alloc([D]) for _ in range(2)]
                xn = AR.alloc([D], BF16)
                st6 = AR.alloc([2, 6])
                mv = AR.alloc([8])
                h2t = [AR.alloc([8, 128], BF16) for _ in range(2)]
                st6a = [AR.alloc([2, 6]) for _ in range(2)]
                mva = [AR.alloc([8]) for _ in range(2)]
                B_smalla = [Buf("lnsmallA0"), Buf("lnsmallA1")]
                rt = AR.alloc([8, 64])
                m8 = AR.alloc([8, 8])
                rsm = AR.alloc([40])
                B_sg = [Buf("sg0"), Buf("sg1")]
                B_m1 = [Buf(f"m1{j}") for j in range(4)]
                B_u = [Buf("u0"), Buf("u1")]
                B_xt = [Buf("xt0"), Buf("xt1")]
                B_xn, B_small = Buf("xn"), Buf("lnsmall")
                B_h2t = [Buf("h2t0"), Buf("h2t1")]
                B_rt, B_m8, B_rsm = Buf("rt"), Buf("m8"), Buf("rsm")
                for half in range(2):
                    wga, bga = load_w(win_cols(C_GA + half * 512, 512), (8, 512))
                    for j in range(4):
                        ps, pb = next_ps()
                        proj_fm(wga, bga, j, ps, pb)
                        T.op("act", lambda h, ps=ps, j=j: h.activation(out=m1[j], in_=ps, func=AF.Sigmoid), reads=[pb], writes=[B_m1[j]])
                    wso = []
                    for pair in range(2):
                        c0 = half * 512 + pair * 256
                        wso.append(load_w(w_sso_d[:, c0:c0 + 256].rearrange("(kc p) n -> p kc n", p=128), (16, 256)))
                    for j in range(4):
                        wv, wb = wso[j // 2]
                        ps2, pb2 = next_ps()

                        def mma(h, ps2=ps2, wv=wv, j=j):
                            for kc in range(16):
                                r = h.matmul(ps2, wv[:, kc, (j % 2) * 128:(j % 2 + 1) * 128], gnT[:, kc, :], start=(kc == 0), stop=(kc == 15))
                            return r
                        T.op("pe", mma, reads=[wb, B_xsT], writes=[pb2])
                        T.op("dve", lambda h, ps2=ps2, j=j: h.tensor_tensor(out=m1[j], in0=ps2, in1=m1[j], op=ALU.mult),
                             reads=[pb2, B_m1[j]], writes=[B_m1[j]])
                    wgb, bgb = load_w(win_cols(C_GB + half * 512, 512), (8, 512))
                    wsc, bsc = load_w(w_sco_d[:, half * 512:(half + 1) * 512].rearrange("(kc p) n -> p kc n", p=128), (8, 512))
                    for j in range(4):
                        dj = half * 4 + j
                        sl = j % 2
                        ps3, pb3 = next_ps()
                        proj_fm(wgb, bgb, j, ps3, pb3)
                        T.op("act", lambda h, ps3=ps3, sl=sl: h.activation(out=sg[sl], in_=ps3, func=AF.Sigmoid), reads=[pb3], writes=[B_sg[sl]])
                        ps4, pb4 = next_ps()

                        def mmb(h, ps4=ps4, wsc=wsc, j=j):
                            for kc in range(8):
                                r = h.matmul(ps4, wsc[:, kc, j * 128:(j + 1) * 128], ubT[:, kc, :], start=(kc == 0), stop=(kc == 7))
                            return r
                        T.op("pe", mmb, reads=[bsc, B_ubT], writes=[pb4])
                        T.op("dve", lambda h, ps4=ps4, sl=sl: h.tensor_tensor(out=sg[sl], in0=ps4, in1=sg[sl], op=ALU.mult),
                             reads=[pb4, B_sg[sl]], writes=[B_sg[sl]])
                        T.op("dve", lambda h, sl=sl, dj=dj, j=j: h.tensor_tensor(out=mergedT[:, dj, :], in0=m1[j], in1=sg[sl], op=ALU.add),
                             reads=[B_m1[j], B_sg[sl]], writes=B_Btoks)
                if t == 0:
                    tap("mergedT", mergedT, B_Btoks[3])
                wo0, bo0 = load_w(w_o_d[:, 0:512].rearrange("(kc p) n -> p kc n", p=128), (8, 512))
                wo1, bo1 = load_w(w_o_d[:, 512:1024].rearrange("(kc p) n -> p kc n", p=128), (8, 512))

                def partA(ci):
                    chunk = t * 4 + ci
                    tok0 = chunk * 128
                    sl = ci % 2
                    T.dma("sp", [lambda h: h.dma_start(out=xt[sl], in_=x_d[tok0:tok0 + 128, :])], x_sem[sl], writes=[B_xt[sl]])
                    yield
                    for hf, (wo, bo) in enumerate(((wo0, bo0), (wo1, bo1))):
                        ps, pb = next_ps()

                        def mmo(h, ps=ps, wo=wo):
                            for kc in range(8):
                                r = h.matmul(ps, mergedT[:, kc, ci * 128:(ci + 1) * 128], wo[:, kc, :], start=(kc == 0), stop=(kc == 7))
                            return r
                        T.op("pe", mmo, reads=[bo] + B_Btoks, writes=[pb])
                        yield
                        T.op("dve", lambda h, ps=ps, hf=hf: h.tensor_tensor(
                            out=u[sl][:, hf * 512:(hf + 1) * 512], in0=ps, in1=g1b[:, hf * 512:(hf + 1) * 512], op=ALU.mult),
                            reads=[pb, B_g1b], writes=[B_u[sl]])
                        yield
                    if t == 0 and ci == 1:
                        tap("gmix1", u[sl], B_u[sl])
                    T.op("dve", lambda h: h.scalar_tensor_tensor(out=u[sl], in0=xt[sl], scalar=float(ALPHA), in1=u[sl], op0=ALU.mult, op1=ALU.add),
                         reads=[B_xt[sl], B_u[sl]], writes=[B_u[sl]])
                    yield
                    yield from ln_stats_g(u[sl], B_u[sl], st6a[sl], mva[sl], mva[sl][:, 3:4], B_smalla[sl])
                    T.op("dve", lambda h: h.tensor_scalar(out=u[sl], in0=u[sl], scalar1=mva[sl][:, 0:1], scalar2=mva[sl][:, 3:4], op0=ALU.subtract, op1=ALU.mult),
                         reads=[B_u[sl], B_smalla[sl]], writes=[B_u[sl]])
                    yield
                    T.op("dve", lambda h: h.tensor_tensor(out=u[sl], in0=u[sl], in1=rowv[:, R_L1G:R_L1G + D], op=ALU.mult), reads=[B_u[sl], B_rowv], writes=[B_u[sl]])
                    yield
                    T.op("dve", lambda h: h.tensor_tensor(out=u[sl], in0=u[sl], in1=rowv[:, R_L1B:R_L1B + D], op=ALU.add), reads=[B_u[sl], B_rowv], writes=[B_u[sl]])
                    yield
                    T.dma("sp", [lambda h: h.dma_start(out=x1_d[tok0:tok0 + 128, :], in_=u[sl])], x1st_sem[sl], reads=[B_u[sl]])
                    yield
                    if t == 0 and ci == 1:
                        tap("x1_1", u[sl], B_u[sl])

                def partB(ci):
                    chunk = t * 4 + ci
                    tok0 = chunk * 128
                    sl = ci % 2
                    yield from ln_mod_T_g(u[sl], B_u[sl], xn, B_xn, st6, mv, B_small, 16, lambda kc: h2t[sl][:, kc, :], B_h2t[sl])
                    T.dma("sp", [lambda h: h.dma_start(out=h2T_d[:, :, tok0:tok0 + 128], in_=h2t[sl])], h2st_sem[sl], reads=[B_h2t[sl]])
                    yield
                    if t == 0 and ci == 1:
                        tap("h2t1", h2t[sl], B_h2t[sl])
                    ps, pb = next_ps()

                    def mmr(h):
                        for kc in range(8):
                            r = h.matmul(ps[:, 0:64], h2t[sl][:, kc, :], rw[:, kc, :], start=(kc == 0), stop=(kc == 7))
                        return r
                    T.op("pe", mmr, reads=[B_h2t[sl], B_rw], writes=[pb])
                    yield
                    sc_, bi_, mk_, se_ = rt[:, 0, :], rt[:, 1, :], rt[:, 2, :], rt[:, 3, :]
                    T.op("act", lambda h: h.activation(out=sc_, in_=ps[:, 0:64], func=AF.Sigmoid), reads=[pb], writes=[B_rt])
                    yield
                    T.op("dve", lambda h: h.tensor_tensor(out=bi_, in0=sc_, in1=rowv[:, R_RB:R_RB + 64], op=ALU.add), reads=[B_rt, B_rowv], writes=[B_rt])
                    yield
                    for g in range(8):
                        T.op("dve", lambda h, g=g: h.max(out=m8[:, g, :], in_=bi_[:, g * 8:(g + 1) * 8]), reads=[B_rt], writes=[B_m8])
                        yield
                    T.op("dve", lambda h: h.tensor_tensor(out=rsm[:, 0:8], in0=m8[:, :, 0], in1=m8[:, :, 1], op=ALU.add), reads=[B_m8], writes=[B_rsm])
                    yield
                    T.op("dve", lambda h: h.max(out=rsm[:, 8:16], in_=rsm[:, 0:8]), reads=[B_rsm], writes=[B_rsm])
                    yield
                    T.op("dve", lambda h: h.tensor_scalar(out=rsm[:, 16:24], in0=rsm[:, 0:8], scalar1=rsm[:, 11:12], scalar2=None, op0=ALU.is_ge),
                         reads=[B_rsm], writes=[B_rsm])
                    yield
                    T.op("dve", lambda h: h.tensor_scalar(out=rsm[:, 24:32], in0=rsm[:, 16:24], scalar1=-1.0, scalar2=1.0e9, op0=ALU.add, op1=ALU.mult),
                         reads=[B_rsm], writes=[B_rsm])
                    yield
                    mk3 = mk_.rearrange("p (a b) -> p a b", a=8)
                    T.op("dve", lambda h: h.tensor_tensor(
                        out=mk3, in0=bi_.rearrange("p (a b) -> p a b", a=8), in1=rsm[:, 16:24].unsqueeze(2).to_broadcast([128, 8, 8]), op=ALU.mult),
                        reads=[B_rt, B_rsm], writes=[B_rt])
                    yield
                    T.op("dve", lambda h: h.tensor_tensor(
                        out=mk3, in0=mk3, in1=rsm[:, 24:32].unsqueeze(2).to_broadcast([128, 8, 8]), op=ALU.add), reads=[B_rt, B_rsm], writes=[B_rt])
                    yield
                    T.op("dve", lambda h: h.max(out=rsm[:, 8:16], in_=mk_), reads=[B_rt], writes=[B_rsm])
                    yield
                    T.op("dve", lambda h: h.tensor_scalar(out=se_, in0=mk_, scalar1=rsm[:, 15:16], scalar2=None, op0=ALU.is_ge), reads=[B_rt, B_rsm], writes=[B_rt])
                    yield
                    T.op("dve", lambda h: h.tensor_tensor(out=se_, in0=se_, in1=sc_, op=ALU.mult), reads=[B_rt], writes=[B_rt])
                    yield
                    T.op("dve", lambda h: h.tensor_reduce(out=rsm[:, 32:33], in_=se_, axis=AX.X, op=ALU.add), reads=[B_rt], writes=[B_rsm])
                    yield
                    T.op("dve", lambda h: h.reciprocal(out=rsm[:, 33:34], in_=rsm[:, 32:33]), reads=[B_rsm], writes=[B_rsm])
                    yield
                    T.op("dve", lambda h: h.tensor_scalar(
                        out=G_all[:, chunk, 0:64], in0=se_, scalar1=rsm[:, 33:34], scalar2=2.5, op0=ALU.mult, op1=ALU.mult),
                        reads=[B_rt, B_rsm], writes=[B_G])
                    yield

                for step in range(5):
                    run_interleaved([partA(step) if step < 4 else None, partB(step - 1) if step >= 1 else None])
                T.barrier()
            if stage_O() == 'stop':
                break
        tap("G", G_all, B_G)

        def phase_B():
            T.barrier(final=True)
            AR.top = PERSIST_TOP
            NB = 2 if n_tiles == NTILE else 1
            BT = S // 2 if n_tiles == NTILE else n_tiles * TT
            nsub = BT // 128
            n512 = BT // 512
            h2blk = AR.alloc([8, BT], BF16)
            acc = AR.alloc([nsub, D])
            wgu = [AR.alloc([8, 512], BF16) for _ in range(2)]
            wdn = [AR.alloc([2, D], BF16) for _ in range(2)]
            sgt = [AR.alloc([512]) for _ in range(2)]
            actT = [AR.alloc([2, 512], BF16) for _ in range(2)]
            xt = [AR.alloc([D]) for _ in range(2)]
            st6 = AR.alloc([2, 6])
            mv = AR.alloc([8])
            B_h2blk, B_small = Buf("h2blk"), Buf("lnsmallB")
            B_acc = [Buf(f"acc{i}") for i in range(nsub)]
            B_wgu = [Buf("wgu0"), Buf("wgu1")]
            B_wdn = [Buf("wdn0"), Buf("wdn1")]
            B_sgt = [Buf("sgt0"), Buf("sgt1")]
            B_actT = [Buf("actT0"), Buf("actT1")]
            B_xt = [Buf("xtB0"), Buf("xtB1")]
            wgu_sem = [new_dsem("wgu0"), new_dsem("wgu1")]
            wdn_sem = [new_dsem("wdn0"), new_dsem("wdn1")]
            h2_sem = new_dsem("h2")
            out_sems = [new_dsem(f"out{i}") for i in range(4)]
            B_out = Buf("out")
            for blk in range(NB):
                t0 = blk * BT
                T.dma("sp", [lambda h, t0=t0: h.dma_start(out=h2blk, in_=h2T_d[:, :, t0:t0 + BT])], h2_sem, writes=[B_h2blk])
                T.op("dve", lambda h: h.memset(acc, 0.0), writes=B_acc)
                aslot = 0
                for e in range(n_exp):
                    ee = e if n_exp == NE else (e if e < n_exp - 1 else NE - 1)
                    sl = e % 2
                    T.dma("pool", [
                        lambda h, sl=sl, ee=ee: h.dma_start(out=wgu[sl][:, :, 0:256], in_=wg_d[ee].rearrange("(kc p) n -> p kc n", p=128)),
                        lambda h, sl=sl, ee=ee: h.dma_start(out=wgu[sl][:, :, 256:512], in_=wu_d[ee].rearrange("(kc p) n -> p kc n", p=128)),
                    ], wgu_sem[sl], writes=[B_wgu[sl]])
                    T.dma("pool", [lambda h, sl=sl, ee=ee: h.dma_start(out=wdn[sl], in_=wd_d[ee].rearrange("(fc p) n -> p fc n", p=128))],
                          wdn_sem[sl], writes=[B_wdn[sl]])
                    for t4 in range(n512):
                        ts_ = slice(t4 * 512, (t4 + 1) * 512)
                        asl = aslot % 2
                        aslot += 1
                        for fc in range(2):
                            ssl = fc
                            ps_g, pb_g = next_ps()
                            ps_u, pb_u = next_ps()

                            def mmg(h, ps_g=ps_g, sl=sl, fc=fc, ts_=ts_):
                                for kc in range(8):
                                    r = h.matmul(ps_g, wgu[sl][:, kc, fc * 128:(fc + 1) * 128], h2blk[:, kc, ts_], start=(kc == 0), stop=(kc == 7))
                                return r

                            def mmu(h, ps_u=ps_u, sl=sl, fc=fc, ts_=ts_):
                                for kc in range(8):
                                    r = h.matmul(ps_u, wgu[sl][:, kc, 256 + fc * 128:256 + (fc + 1) * 128], h2blk[:, kc, ts_], start=(kc == 0), stop=(kc == 7))
                                return r
                            T.op("pe", mmg, reads=[B_wgu[sl], B_h2blk], writes=[pb_g])
                            T.op("pe", mmu, reads=[B_wgu[sl], B_h2blk], writes=[pb_u])
                            T.op("act", lambda h, ps_g=ps_g, ssl=ssl: h.activation(out=sgt[ssl], in_=ps_g, func=AF.Silu), reads=[pb_g], writes=[B_sgt[ssl]])
                            T.op("dve", lambda h, ps_u=ps_u, ssl=ssl, asl=asl, fc=fc: h.tensor_tensor(
                                out=actT[asl][:, fc, :], in0=ps_u, in1=sgt[ssl], op=ALU.mult), reads=[pb_u, B_sgt[ssl]], writes=[B_actT[asl]])
                        for sub in range(4):
                            ti = t4 * 4 + sub
                            gcol = G_all[:, blk * nsub + ti, ee:ee + 1]
                            for hf in range(2):
                                ps_d, pb_d = next_ps()

                                def mmd(h, ps_d=ps_d, asl=asl, sub=sub, sl=sl, hf=hf):
                                    for fc in range(2):
                                        r = h.matmul(ps_d, actT[asl][:, fc, sub * 128:(sub + 1) * 128], wdn[sl][:, fc, hf * 512:(hf + 1) * 512],
                                                     start=(fc == 0), stop=(fc == 1))
                                    return r
                                T.op("pe", mmd, reads=[B_actT[asl], B_wdn[sl]], writes=[pb_d])
                                T.op("dve", lambda h, ps_d=ps_d, ti=ti, hf=hf, gcol=gcol: h.scalar_tensor_tensor(
                                    out=acc[:, ti, hf * 512:(hf + 1) * 512], in0=ps_d, scalar=gcol, in1=acc[:, ti, hf * 512:(hf + 1) * 512],
                                    op0=ALU.mult, op1=ALU.add), reads=[pb_d, B_G, B_acc[ti]], writes=[B_acc[ti]])
                if blk == 0:
                    tap("ffn1", acc[:, 1, :], B_acc[1])
                for ti in range(nsub):
                    tok0 = t0 + ti * 128
                    sl = ti % 2
                    T.dma("sp", [lambda h, sl=sl, tok0=tok0: h.dma_start(out=xt[sl], in_=x1_d[tok0:tok0 + 128, :])], x_sem[sl], writes=[B_xt[sl]])
                    a = acc[:, ti, :]
                    T.op("dve", lambda h, a=a: h.tensor_tensor(out=a, in0=a, in1=g2b, op=ALU.mult), reads=[B_acc[ti], B_g2b], writes=[B_acc[ti]])
                    T.op("dve", lambda h, a=a, sl=sl: h.scalar_tensor_tensor(out=a, in0=xt[sl], scalar=float(ALPHA), in1=a, op0=ALU.mult, op1=ALU.add),
                         reads=[B_xt[sl], B_acc[ti]], writes=[B_acc[ti]])
                    ln_stats(a, B_acc[ti], st6, mv, mv[:, 3:4], B_small)
                    T.op("dve", lambda h, a=a: h.tensor_scalar(out=a, in0=a, scalar1=mv[:, 0:1], scalar2=mv[:, 3:4], op0=ALU.subtract, op1=ALU.mult),
                         reads=[B_acc[ti], B_small], writes=[B_acc[ti]])
                    T.op("dve", lambda h, a=a: h.tensor_tensor(out=a, in0=a, in1=rowv[:, R_L2G:R_L2G + D], op=ALU.mult), reads=[B_acc[ti], B_rowv], writes=[B_acc[ti]])
                    T.op("dve", lambda h, a=a: h.tensor_tensor(out=a, in0=a, in1=rowv[:, R_L2B:R_L2B + D], op=ALU.add), reads=[B_acc[ti], B_rowv], writes=[B_acc[ti]])
                    T.dma("sp", [lambda h, a=a, tok0=tok0: h.dma_start(out=out_d[tok0:tok0 + 128, :], in_=a)], out_sems[ti % 4], reads=[B_acc[ti]], writes=[B_out])
                T.barrier()
        if do_moe:
            phase_B()
        T.barrier(final=True)
        with nc.Block() as block:
            T.emit(block)
    return nc, tap_out


def _host_pack(inputs, b):
    f = np.float32
    g = lambda k: np.asarray(inputs[k], dtype=f)
    consts = np.zeros((128, NCONST), f)
    consts[:, K_ID:K_ID + 128] = np.eye(128, dtype=f)
    jj, ll = np.meshgrid(np.arange(128), np.arange(128), indexing="ij")
    consts[:, K_TRIU:K_TRIU + 128] = (jj <= ll)
    consts[:, K_STRIL:K_STRIL + 128] = (jj > ll)
    consts[:, K_ONES:K_ONES + 128] = 1.0
    consts[:, K_EPSLN] = LN_EPS
    consts[:, K_EPSRMS] = RMS_EPS
    consts[:, K_ONE] = 1.0
    rowv = np.concatenate([g("ssd_dt_bias")[0], g("ssd_A_log")[0], g("ssd_D")[0], g("router_bias")[0], g("ssd_norm_w")[0],
                           g("ln1_g")[0], g("ln1_b")[0], g("ln2_g")[0], g("ln2_b")[0]])[None, :]
    assert rowv.shape[1] == NROW
    cw = g("ssd_conv_w")[0].reshape(4, 32, 128).transpose(2, 1, 0).reshape(128, 128)
    cb = g("ssd_conv_b")[0].reshape(32, 128).T
    scw = g("sc_conv_w")[0].reshape(3, 8, 128).transpose(2, 1, 0).reshape(128, 24)
    colv = np.ascontiguousarray(np.concatenate([cw, cb, scw], axis=1))
    assert colv.shape[1] == NCOL
    m = {
        "x": np.ascontiguousarray(g("x")[b]),
        "cT": np.ascontiguousarray(g("c")[b].reshape(8, 128).T),
        "w_ada": g("w_ada")[0], "b_ada": g("b_ada")[0][None, :], "w_in": g("w_in")[0],
        "colv": colv, "rowv": np.ascontiguousarray(rowv), "consts": consts,
        "w_ssd_out": g("w_ssd_out")[0], "w_sc_out": g("w_sc_out")[0], "w_o": g("w_o")[0], "router_w": g("router_w")[0],
    }
    return m


_SHARED = {}


def _shared_pack(inputs):
    f = np.float32
    g = lambda k: np.asarray(inputs[k], dtype=f)
    return {
        "w_gate": np.concatenate([g("w_gate")[0], g("sh_gate")], axis=0),
        "w_up": np.concatenate([g("w_up")[0], g("sh_up")], axis=0),
        "w_down": np.concatenate([g("w_down")[0], g("sh_down")], axis=0),
    }


def kernel(**inputs):
    nc, _ = build()
    shared = _shared_pack(inputs)
    in_maps = []
    for b in range(8):
        m = _host_pack(inputs, b)
        m.update(shared)
        in_maps.append(m)
    res = run_bass_kernel_spmd(nc, in_maps, core_ids=list(range(8)))
    return np.stack([np.asarray(r["out"], dtype=np.float32) for r in res.results], axis=0)
```

```python
import numpy as np
from contextlib import ExitStack
import concourse.bass as bass
import concourse.mybir as mybir
from concourse.bass_utils import run_bass_kernel_spmd

F32 = mybir.dt.float32
BF16 = mybir.dt.bfloat16
AF = mybir.ActivationFunctionType
ALU = mybir.AluOpType
AX = mybir.AxisListType

S = 4096
D = 1024
TT = 512
NTILE = S // TT
NE = 65
ALPHA = 2.0 ** 0.25
LN_EPS = 1e-5
RMS_EPS = 1e-5
INCOLS = 11296
C_Z, C_XBC, C_DT, C_SCB, C_SCC, C_SCH, C_GA, C_GB = 0, 2048, 6144, 6176, 7200, 8224, 9248, 10272
R_DTB, R_ALOG, R_D, R_RB, R_NW, R_L1G, R_L1B, R_L2G, R_L2B = 0, 32, 64, 96, 160, 2208, 3232, 4256, 5280
NROW = 6304
CV_CW, CV_CB, CV_SCW = 0, 128, 160
NCOL = 184
K_ID, K_TRIU, K_STRIL, K_ONES, K_EPSLN, K_EPSRMS, K_ONE = 0, 128, 256, 384, 512, 513, 514
NCONST = 516
ARENA_WORDS = 52600


class Buf:
    __slots__ = ("name", "w", "r")

    def __init__(self, name):
        self.name = name
        self.w = None
        self.r = {}


class Trk:
    ENGS = ("pe", "act", "dve", "pool", "sp")

    def __init__(self, nc, stack):
        self.nc = nc
        self.stack = stack
        self.sem = {}
        self.cnt = {}
        self.q = {e: [] for e in self.ENGS}
        self.seen = {e: {} for e in self.ENGS}
        for e in self.ENGS:
            self.newsem("E_" + e)

    def newsem(self, name):
        self.sem[name] = self.stack.enter_context(self.nc.semaphore(name))
        self.cnt[name] = 0
        return name

    def _waits(self, eng, reads, writes):
        need = {}
        own = "E_" + eng

        def add(s, v):
            if v > need.get(s, 0):
                need[s] = v
        for b in reads:
            if b.w:
                add(*b.w)
        for b in writes:
            if b.w:
                add(*b.w)
            for s, v in b.r.items():
                if s != own:
                    add(s, v)
        out = []
        for s, v in need.items():
            if s == own and eng == "pe":
                continue
            if self.seen[eng].get(s, 0) >= v:
                continue
            self.seen[eng][s] = v
            out.append((s, v))
        return out

    def op(self, eng, fn, reads=(), writes=()):
        w = self._waits(eng, reads, writes)
        s = "E_" + eng
        self.cnt[s] += 1
        v = self.cnt[s]
        self.q[eng].append((w, fn, s, 1))
        for b in reads:
            b.r[s] = v
        for b in writes:
            b.w = (s, v)
            b.r = {}

    def dma(self, eng, fns, dsem, reads=(), writes=()):
        w = self._waits(eng, reads, writes)
        for i, fn in enumerate(fns):
            self.q[eng].append((w if i == 0 else [], fn, dsem, 16))
        self.cnt[dsem] += 16 * len(fns)
        v = self.cnt[dsem]
        for b in reads:
            b.r[dsem] = v
        for b in writes:
            b.w = (dsem, v)
            b.r = {}

    def barrier(self, final=False):
        snap = dict(self.cnt)
        for e in self.ENGS:
            if e == "pool" and not final:
                continue
            w = []
            for s, v in snap.items():
                if v > 0 and self.seen[e].get(s, 0) < v and not (s == "E_" + e and e == "pe"):
                    self.seen[e][s] = v
                    w.append((s, v))
            if w:
                self.q[e].append((w, None, None, 0))

    def emit(self, block):
        sem = self.sem

        def run(h, q):
            for waits, fn, s, inc in q:
                for ws, wv in waits:
                    h.wait_ge(sem[ws], wv)
                if fn is not None:
                    fn(h).then_inc(sem[s], inc)

        @block.tensor
        def _(h):
            run(h, self.q["pe"])

        @block.scalar
        def _(h):
            run(h, self.q["act"])

        @block.vector
        def _(h):
            run(h, self.q["dve"])

        @block.gpsimd
        def _(h):
            run(h, self.q["pool"])

        @block.sync
        def _(h):
            run(h, self.q["sp"])


class Arena:
    def __init__(self, ap, nwords):
        self.ap = ap
        self.n = nwords
        self.top = 0

    def alloc(self, shape, dtype=F32):
        n = int(np.prod(shape))
        words = n if dtype == F32 else (n + 1) // 2
        off = self.top
        self.top += words
        assert self.top <= self.n, ("arena overflow", self.top, self.n)
        v = self.ap[:, off:off + words]
        if dtype == BF16:
            v = v.bitcast(BF16)
        if len(shape) == 2:
            v = v.rearrange("p (a b) -> p a b", a=shape[0])
        elif len(shape) == 3:
            v = v.rearrange("p (a b c) -> p a b c", a=shape[0], b=shape[1])
        return v


def build(cfg=None):
    cfg = cfg or {}
    n_tiles = cfg.get("n_tiles", NTILE)
    do_moe = cfg.get("moe", True)
    n_exp = cfg.get("n_exp", NE)
    taps = cfg.get("taps", ())
    nc = bass.Bass("TRN2", target_bir_lowering=False)

    def din(name, shape, dt=F32):
        return nc.dram_tensor(name, list(shape), dt, kind="ExternalInput").ap()

    x_d = din("x", [S, D])
    cT_d = din("cT", [128, 8])
    w_ada_d = din("w_ada", [D, 6 * D])
    b_ada_d = din("b_ada", [1, 6 * D])
    w_in_d = din("w_in", [D, INCOLS])
    colv_d = din("colv", [128, NCOL])
    rowv_d = din("rowv", [1, NROW])
    cst_d = din("consts", [128, NCONST])
    w_sso_d = din("w_ssd_out", [2048, D])
    w_sco_d = din("w_sc_out", [D, D])
    w_o_d = din("w_o", [D, D])
    rw_d = din("router_w", [D, 64])
    wg_d = din("w_gate", [NE, D, 256])
    wu_d = din("w_up", [NE, D, 256])
    wd_d = din("w_down", [NE, 256, D])
    out_d = nc.dram_tensor("out", [S, D], F32, kind="ExternalOutput").ap()
    x1_d = nc.dram_tensor("x1_scr", [S, D], F32, kind="Internal").ap()
    h2T_d = nc.dram_tensor("h2T_scr", [128, 8, S], BF16, kind="Internal").ap()

    tap_out = {}
    with ExitStack() as stack:
        arena_t = stack.enter_context(nc.sbuf_tensor("arena", [128, ARENA_WORDS], F32))
        ps_t = [stack.enter_context(nc.psum_tensor(f"ps{i}", [128, 512], F32)) for i in range(8)]
        T = Trk(nc, stack)
        AR = Arena(arena_t[:], ARENA_WORDS)
        PSB = [Buf(f"ps{i}") for i in range(8)]
        ps_rr = [0]

        def next_ps():
            i = ps_rr[0] % 8
            ps_rr[0] += 1
            return ps_t[i][:], PSB[i]

        dsem_n = [0]

        def new_dsem(tag):
            dsem_n[0] += 1
            return T.newsem(f"D{dsem_n[0]}_{tag}")

        tap_sem = new_dsem("tap")

        def tap(name, ap, buf):
            if name not in taps:
                return
            dt = ap.dtype
            t = nc.dram_tensor("tap_" + name, list(ap.shape), dt, kind="ExternalOutput").ap()
            tap_out[name] = t
            T.dma("sp", [lambda h, t=t, ap=ap: h.dma_start(out=t, in_=ap)], tap_sem, reads=[buf], writes=[])

        cst = AR.alloc([NCONST])
        identF = cst[:, K_ID:K_ID + 128]
        triU = cst[:, K_TRIU:K_TRIU + 128]
        striL = cst[:, K_STRIL:K_STRIL + 128]
        onesF = cst[:, K_ONES:K_ONES + 128]
        epsln = cst[:, K_EPSLN:K_EPSLN + 1]
        epsrms = cst[:, K_EPSRMS:K_EPSRMS + 1]
        onecol = cst[:, K_ONE:K_ONE + 1]
        identB = AR.alloc([128], BF16)
        rowv = AR.alloc([NROW])
        colv = AR.alloc([NCOL])
        modcol = AR.alloc([32])
        g1b = AR.alloc([D])
        g2b = AR.alloc([D])
        negA = AR.alloc([32])
        G_all = AR.alloc([32, NE])
        rw = AR.alloc([8, 64], BF16)
        halo = AR.alloc([32, 3])
        halo2 = AR.alloc([8, 2])
        state = AR.alloc([32, 64])
        stbf = AR.alloc([32, 64], BF16)
        B_cst, B_identB, B_rowv, B_colv, B_modcol, B_g1b, B_g2b, B_negA, B_G, B_rw = (
            Buf(n) for n in ("cst", "identB", "rowv", "colv", "modcol", "g1b", "g2b", "negA", "G", "rw"))
        B_halo = [Buf(f"halo{i}") for i in range(32)]
        B_halo2 = [Buf(f"halo2_{i}") for i in range(8)]
        B_state = [Buf(f"state{g}") for g in range(8)]
        B_stbf = [Buf(f"stbf{g}") for g in range(8)]
        PERSIST_TOP = AR.top

        ld_sem = new_dsem("ld")
        T.dma("sp", [lambda h: h.dma_start(out=cst, in_=cst_d[:, :])], new_dsem("cst"), writes=[B_cst])
        T.dma("sp", [lambda h: h.dma_start(out=rowv, in_=rowv_d[0, :].partition_broadcast(128))], new_dsem("rowv"), writes=[B_rowv])
        T.dma("sp", [lambda h: h.dma_start(out=colv, in_=colv_d[:, :])], new_dsem("colv"), writes=[B_colv])
        rw_sem = new_dsem("rw")
        T.dma("pool", [lambda h: h.dma_start(out=rw, in_=rw_d.rearrange("(kc p) n -> p kc n", p=128))], rw_sem, writes=[B_rw])
        T.op("dve", lambda h: h.tensor_copy(out=identB, in_=identF), reads=[B_cst], writes=[B_identB])
        T.op("act", lambda h: h.activation(out=negA, in_=rowv[:, R_ALOG:R_ALOG + 32], func=AF.Exp), reads=[B_rowv], writes=[B_negA])
        T.op("dve", lambda h: h.tensor_scalar(out=negA, in0=negA, scalar1=-1.0, scalar2=None, op0=ALU.mult), reads=[B_negA], writes=[B_negA])
        T.op("dve", lambda h: h.memset(halo, 0.0), writes=B_halo)
        T.op("dve", lambda h: h.memset(halo2, 0.0), writes=B_halo2)
        T.op("dve", lambda h: h.memset(state, 0.0), writes=B_state)
        T.op("dve", lambda h: h.memset(stbf, 0.0), writes=B_stbf)
        T.op("dve", lambda h: h.memset(G_all, 1.0), writes=[B_G])

        cTt = AR.alloc([8])
        scT = AR.alloc([8])
        modrow = AR.alloc([6 * D])
        badar = AR.alloc([6 * D])
        wa = [AR.alloc([8, 512]) for _ in range(2)]
        B_cT, B_scT, B_modrow, B_badar = Buf("cT"), Buf("scT"), Buf("modrow"), Buf("badar")
        B_wa = [Buf("wa0"), Buf("wa1")]
        wa_sem = [new_dsem("wa0"), new_dsem("wa1")]
        T.dma("sp", [lambda h: h.dma_start(out=cTt, in_=cT_d[:, :])], new_dsem("cT"), writes=[B_cT])
        T.dma("sp", [lambda h: h.dma_start(out=badar[0:1, :], in_=b_ada_d[:, :])], new_dsem("bada"), writes=[B_badar])
        T.op("act", lambda h: h.activation(out=scT, in_=cTt, func=AF.Silu), reads=[B_cT], writes=[B_scT])
        for nb in range(12):
            sl = nb % 2
            T.dma("sp", [lambda h, nb=nb, sl=sl: h.dma_start(
                out=wa[sl], in_=w_ada_d[:, nb * 512:(nb + 1) * 512].rearrange("(kc p) n -> p kc n", p=128))],
                wa_sem[sl], writes=[B_wa[sl]])
            ps, pb = next_ps()

            def mm(h, ps=ps, sl=sl):
                for kc in range(8):
                    r = h.matmul(ps[0:1, :], scT[:, kc:kc + 1], wa[sl][:, kc, :], start=(kc == 0), stop=(kc == 7))
                return r
            T.op("pe", mm, reads=[B_scT, B_wa[sl]], writes=[pb])
            T.op("dve", lambda h, ps=ps, nb=nb: h.tensor_tensor(
                out=modrow[0:1, nb * 512:(nb + 1) * 512], in0=ps[0:1, :], in1=badar[0:1, nb * 512:(nb + 1) * 512], op=ALU.add),
                reads=[pb, B_badar], writes=[B_modrow])
        ps, pb = next_ps()

        def mmcol(h, ps=ps):
            for j in range(32):
                off = [0, 1024, 3072, 4096][j // 8] + (j % 8) * 128
                r = h.matmul(ps[:, j:j + 1], modrow[0:1, off:off + 128], onecol[0:1, 0:1], start=True, stop=True)
            return r
        T.op("pe", mmcol, reads=[B_modrow, B_cst], writes=[pb])
        T.op("dve", lambda h, ps=ps: h.tensor_copy(out=modcol, in_=ps[:, 0:32]), reads=[pb], writes=[B_modcol])
        T.op("dve", lambda h: h.tensor_scalar(out=modcol[:, 8:16], in0=modcol[:, 8:16], scalar1=1.0, scalar2=None, op0=ALU.add),
             reads=[B_modcol], writes=[B_modcol])
        T.op("dve", lambda h: h.tensor_scalar(out=modcol[:, 24:32], in0=modcol[:, 24:32], scalar1=1.0, scalar2=None, op0=ALU.add),
             reads=[B_modcol], writes=[B_modcol])
        for gi, (gb_, bb) in enumerate(((g1b, B_g1b), (g2b, B_g2b))):
            for hf in range(2):
                ps, pb = next_ps()
                off = (2048 if gi == 0 else 5120) + hf * 512
                T.op("pe", lambda h, ps=ps, off=off: h.matmul(ps, onesF[0:1, :], modrow[0:1, off:off + 512], start=True, stop=True),
                     reads=[B_modrow, B_cst], writes=[pb])
                T.op("act", lambda h, ps=ps, gb_=gb_, hf=hf: h.copy(out=gb_[:, hf * 512:(hf + 1) * 512], in_=ps),
                     reads=[pb], writes=[bb])
        tap("modrow", modrow[0:1, :], B_modrow)
        tap("modcol", modcol, B_modcol)
        tap("g1b", g1b, B_g1b)
        T.barrier(final=True)
        AR.top = PERSIST_TOP

        def ln_stats_g(src, B_src, st, mv, rstd, B_small):
            T.op("dve", lambda h: h.bn_stats(out=st[:, 0, :], in_=src[:, 0:512]), reads=[B_src], writes=[B_small])
            yield
            T.op("dve", lambda h: h.bn_stats(out=st[:, 1, :], in_=src[:, 512:1024]), reads=[B_src], writes=[B_small])
            yield
            T.op("dve", lambda h: h.bn_aggr(out=mv[:, 0:2], in_=st.rearrange("p a b -> p (a b)")), reads=[B_small], writes=[B_small])
            yield
            T.op("act", lambda h: h.activation(out=mv[:, 2:3], in_=mv[:, 1:2], func=AF.Sqrt, bias=epsln, scale=1.0),
                 reads=[B_small, B_cst], writes=[B_small])
            yield
            T.op("dve", lambda h: h.reciprocal(out=rstd, in_=mv[:, 2:3]), reads=[B_small], writes=[B_small])
            yield

        def ln_stats(*a):
            for _ in ln_stats_g(*a):
                pass

        def ln_mod_T_g(src, B_src, xn, B_xn, st, mv, B_small, colbase, dst_fn, B_dst):
            rstd = mv[:, 3:4]
            yield from ln_stats_g(src, B_src, st, mv, rstd, B_small)
            T.op("dve", lambda h: h.tensor_scalar(out=xn, in0=src, scalar1=mv[:, 0:1], scalar2=rstd, op0=ALU.subtract, op1=ALU.mult),
                 reads=[B_src, B_small], writes=[B_xn])
            yield
            ps, pb = next_ps()
            psb = ps.bitcast(BF16)

            def tr(h):
                for kc in range(8):
                    r = h.transpose(psb[:, kc * 128:(kc + 1) * 128], xn[:, kc * 128:(kc + 1) * 128], identB)
                return r
            T.op("pe", tr, reads=[B_xn, B_identB], writes=[pb])
            yield
            for kc in range(8):
                T.op("act", lambda h, kc=kc: h.activation(
                    out=dst_fn(kc), in_=psb[:, kc * 128:(kc + 1) * 128], func=AF.Identity,
                    scale=modcol[:, colbase + 8 + kc:colbase + 9 + kc], bias=modcol[:, colbase + kc:colbase + kc + 1]),
                    reads=[pb, B_modcol], writes=[B_dst])
                yield

        def ln_mod_T(*a):
            for _ in ln_mod_T_g(*a):
                pass

        def run_interleaved(gens):
            gens = [g for g in gens if g is not None]
            while gens:
                for g in list(gens):
                    try:
                        next(g)
                    except StopIteration:
                        gens.remove(g)

        hT = AR.alloc([8, TT], BF16)
        sz = AR.alloc([4, 2048], BF16)
        xbcT = AR.alloc([32, TT], BF16)
        xs_tok = AR.alloc([4, 2048], BF16)
        B_tok = AR.alloc([4, 1024], BF16)
        mergedT = B_tok.rearrange("p a b -> p (a b)").rearrange("p (a b) -> p a b", a=8)
        ubT = AR.alloc([8, TT], BF16)
        dtt = AR.alloc([4, 32])
        wblk = [AR.alloc([8, 512], BF16) for _ in range(2)]
        B_sz, B_xsT, B_BCT, B_ubT, B_dt = (
            Buf(n) for n in ("sz", "xsT", "BCT", "ubT", "dt"))
        B_hTs = [Buf(f"hT{i}") for i in range(4)]
        B_xstoks = [Buf(f"xstok{i}") for i in range(4)]
        B_Btoks = [Buf(f"Btok{i}") for i in range(4)]
        B_wblk = [Buf("wblk0"), Buf("wblk1")]
        wblk_sem = [new_dsem("wblk0"), new_dsem("wblk1")]
        wrr = [0]
        x_sem = [new_dsem("x0"), new_dsem("x1")]
        x1st_sem = [new_dsem("x1st0"), new_dsem("x1st1")]
        h2st_sem = [new_dsem("h2st0"), new_dsem("h2st1")]
        UNION_TOP = AR.top

        def load_w(src_ap, shape3):
            sl = wrr[0] % 2
            wrr[0] += 1
            flat = wblk[sl].rearrange("p a b -> p (a b)")
            n = shape3[0] * shape3[1]
            v = flat[:, 0:n].rearrange("p (a b) -> p a b", a=shape3[0])
            T.dma("pool", [lambda h, v=v, src_ap=src_ap: h.dma_start(out=v, in_=src_ap)], wblk_sem[sl], writes=[B_wblk[sl]])
            return v, B_wblk[sl]

        def win_cols(c0, n):
            return w_in_d[:, c0:c0 + n].rearrange("(kc p) n -> p kc n", p=128)

        def proj_fm(wv, wb, j, ps, pb):
            def mm(h):
                for kc in range(8):
                    r = h.matmul(ps, wv[:, kc, j * 128:(j + 1) * 128], hT[:, kc, :], start=(kc == 0), stop=(kc == 7))
                return r
            T.op("pe", mm, reads=[wb] + B_hTs, writes=[pb])

        for t in range(n_tiles):
            def stage_P(t=t):
                AR.top = UNION_TOP
                xt = [AR.alloc([D]) for _ in range(2)]
                xn = AR.alloc([D], BF16)
                st6 = AR.alloc([2, 6])
                mv = AR.alloc([8])
                pre = [AR.alloc([TT + 3]) for _ in range(2)]
                preb = [AR.alloc([TT + 4], BF16) for _ in range(2)]
                diag = [AR.alloc([4, 128], BF16) for _ in range(2)]
                sccbuf = AR.alloc([4, TT])
                dtmp = AR.alloc([2, 32])
                B_xt = [Buf("xt0"), Buf("xt1")]
                B_xn, B_small = Buf("xn"), Buf("lnsmall")
                B_pre = [Buf("pre0"), Buf("pre1")]
                B_preb = [Buf("preb0"), Buf("preb1")]
                B_diag = [Buf("diag0"), Buf("diag1")]
                B_scc = [Buf(f"scc{j}") for j in range(4)]
                B_dtmp = Buf("dtmp")
                for ci in range(4):
                    tok0 = t * TT + ci * 128
                    sl = ci % 2
                    T.dma("sp", [lambda h, sl=sl, tok0=tok0: h.dma_start(out=xt[sl], in_=x_d[tok0:tok0 + 128, :])], x_sem[sl], writes=[B_xt[sl]])
                    ln_mod_T(xt[sl], B_xt[sl], xn, B_xn, st6, mv, B_small, 0,
                             lambda kc, ci=ci: hT[:, kc, ci * 128:(ci + 1) * 128], B_hTs[ci])
                    if t == 0 and ci == 0:
                        tap("xt0", xt[sl], B_xt[sl])
                        tap("xn0", xn, B_xn)
                        tap("mv0", mv, B_small)
                        if cfg.get("stop") == "ln0":
                            tap("hT", hT, B_hTs[3])
                            return 'stop'
                if t == 0:
                    tap("hT", hT, B_hTs[3])
                for blk in range(4):
                    wv, wb = load_w(win_cols(C_Z + blk * 512, 512), (8, 512))
                    for ci in range(4):
                        ps, pb = next_ps()

                        def mm(h, ps=ps, wv=wv, ci=ci):
                            for kc in range(8):
                                r = h.matmul(ps, hT[:, kc, ci * 128:(ci + 1) * 128], wv[:, kc, :], start=(kc == 0), stop=(kc == 7))
                            return r
                        T.op("pe", mm, reads=[wb, B_hTs[ci]], writes=[pb])
                        T.op("act", lambda h, ps=ps, ci=ci, blk=blk: h.activation(out=sz[:, ci, blk * 512:(blk + 1) * 512], in_=ps, func=AF.Silu),
                             reads=[pb], writes=[B_sz])
                wcur = [None]

                def xbc_front(idx):
                    blk, j = idx // 4, idx % 4
                    if j == 0:
                        wcur[0] = load_w(win_cols(C_XBC + blk * 512, 512), (8, 512))
                    wv, wb = wcur[0]
                    ch = idx
                    sl = idx % 2
                    ps, pb = next_ps()
                    proj_fm(wv, wb, j, ps, pb)
                    T.op("act", lambda h: h.copy(out=preb[sl][:, 3:TT + 3], in_=ps), reads=[pb], writes=[B_preb[sl]])
                    T.op("dve", lambda h: h.tensor_copy(out=preb[sl][:, 0:3], in_=halo[:, ch, :]), reads=[B_halo[ch]], writes=[B_preb[sl]])
                    T.op("dve", lambda h: h.tensor_copy(out=halo[:, ch, :], in_=preb[sl][:, TT:TT + 3]), reads=[B_preb[sl]], writes=[B_halo[ch]])
                    cw0 = CV_CW + ch * 4
                    for k in range(4):
                        T.op("dve", lambda h, k=k: h.tensor_scalar(
                            out=diag[sl][:, k, :], in0=identB, scalar1=colv[:, cw0 + k:cw0 + k + 1], scalar2=None, op0=ALU.mult),
                            reads=[B_identB, B_colv], writes=[B_diag[sl]])

                def xbc_back(idx):
                    ch = idx
                    sl = idx % 2
                    ps2, pb2 = next_ps()

                    def mmc(h):
                        for k in range(4):
                            r = h.matmul(ps2, diag[sl][:, k, :], preb[sl][:, k:k + TT], start=(k == 0), stop=(k == 3))
                        return r
                    T.op("pe", mmc, reads=[B_diag[sl], B_preb[sl]], writes=[pb2])
                    T.op("act", lambda h: h.activation(
                        out=xbcT[:, ch, :], in_=ps2, func=AF.Silu, bias=colv[:, CV_CB + ch:CV_CB + ch + 1], scale=1.0),
                        reads=[pb2, B_colv], writes=[B_xsT if ch < 16 else B_BCT])

                xbc_front(0)
                for idx in range(32):
                    if idx + 1 < 32:
                        xbc_front(idx + 1)
                    xbc_back(idx)
                wv, wb = load_w(win_cols(C_DT, 32), (8, 32))
                for ci in range(4):
                    ps, pb = next_ps()

                    def mm(h, ps=ps, wv=wv, ci=ci):
                        for kc in range(8):
                            r = h.matmul(ps[:, 0:32], hT[:, kc, ci * 128:(ci + 1) * 128], wv[:, kc, :], start=(kc == 0), stop=(kc == 7))
                        return r
                    T.op("pe", mm, reads=[wb, B_hTs[ci]], writes=[pb])
                    T.op("dve", lambda h, ps=ps: h.tensor_tensor(out=dtmp[:, 0, :], in0=ps[:, 0:32], in1=rowv[:, R_DTB:R_DTB + 32], op=ALU.add),
                         reads=[pb, B_rowv], writes=[B_dtmp])
                    T.op("act", lambda h: h.activation(out=dtmp[:, 1, :], in_=dtmp[:, 0, :], func=AF.Exp), reads=[B_dtmp], writes=[B_dtmp])
                    T.op("act", lambda h, ci=ci: h.activation(out=dtt[:, ci, :], in_=dtmp[:, 1, :], func=AF.Ln, bias=onecol, scale=1.0),
                         reads=[B_dtmp, B_cst], writes=[B_dt])
                for k2 in range(2):
                    wv, wb = load_w(win_cols(C_SCC + k2 * 512, 512), (8, 512))
                    for j in range(4):
                        ps, pb = next_ps()
                        proj_fm(wv, wb, j, ps, pb)
                        T.op("act", lambda h, ps=ps, j=j: h.copy(out=sccbuf[:, j, :], in_=ps), reads=[pb], writes=[B_scc[j]])
                    wv, wb = load_w(win_cols(C_SCH + k2 * 512, 512), (8, 512))
                    for j in range(4):
                        ch8 = k2 * 4 + j
                        ps, pb = next_ps()
                        proj_fm(wv, wb, j, ps, pb)
                        sl = j % 2
                        T.op("dve", lambda h, ps=ps, j=j, sl=sl: h.tensor_tensor(out=pre[sl][:, 2:TT + 2], in0=ps, in1=sccbuf[:, j, :], op=ALU.mult),
                             reads=[pb, B_scc[j]], writes=[B_pre[sl]])
                        T.op("dve", lambda h, sl=sl, ch8=ch8: h.tensor_copy(out=pre[sl][:, 0:2], in_=halo2[:, ch8, :]), reads=[B_halo2[ch8]], writes=[B_pre[sl]])
                        T.op("dve", lambda h, sl=sl, ch8=ch8: h.tensor_copy(out=halo2[:, ch8, :], in_=pre[sl][:, TT:TT + 2]), reads=[B_pre[sl]], writes=[B_halo2[ch8]])
                        c0 = CV_SCW + ch8 * 3
                        T.op("dve", lambda h, sl=sl, j=j, c0=c0: h.tensor_scalar(
                            out=sccbuf[:, j, :], in0=pre[sl][:, 0:TT], scalar1=colv[:, c0:c0 + 1], scalar2=None, op0=ALU.mult),
                            reads=[B_pre[sl], B_colv], writes=[B_scc[j]])
                        for k in range(1, 3):
                            T.op("dve", lambda h, sl=sl, j=j, k=k, c0=c0: h.scalar_tensor_tensor(
                                out=sccbuf[:, j, :], in0=pre[sl][:, k:k + TT], scalar=colv[:, c0 + k:c0 + k + 1], in1=sccbuf[:, j, :],
                                op0=ALU.mult, op1=ALU.add), reads=[B_pre[sl], B_colv, B_scc[j]], writes=[B_scc[j]])
                    wv, wb = load_w(win_cols(C_SCB + k2 * 512, 512), (8, 512))
                    for j in range(4):
                        ch8 = k2 * 4 + j
                        ps, pb = next_ps()
                        proj_fm(wv, wb, j, ps, pb)
                        T.op("dve", lambda h, ps=ps, j=j, ch8=ch8: h.tensor_tensor(out=ubT[:, ch8, :], in0=ps, in1=sccbuf[:, j, :], op=ALU.mult),
                             reads=[pb, B_scc[j]], writes=[B_ubT])
                if t == 0:
                    tap("hT_end", hT, B_hTs[3])
                    tap("sz", sz, B_sz)
                    tap("xsT", xbcT[:, 0:16, :], B_xsT)
                    tap("BCT", xbcT[:, 16:32, :], B_BCT)
                    tap("dt", dtt, B_dt)
                    tap("ubT", ubT, B_ubT)
                T.barrier()
                if cfg.get("stop") == "P":
                    return 'stop'
                return None

            if stage_P() == 'stop':
                break
            def stage_S(t=t):
                AR.top = UNION_TOP
                dA = AR.alloc([32])
                sm = AR.alloc([6, 32])
                triDA = [AR.alloc([4, 128]) for _ in range(3)]
                cbm = AR.alloc([8, 128])
                LT = [AR.alloc([4, 128]) for _ in range(2)]
                scTt = [AR.alloc([4, 128], BF16) for _ in range(2)]
                gtmp = AR.alloc([2048])
                Xb = gtmp[:, 0:1024].bitcast(BF16).rearrange("p (a b) -> p a b", a=32)
                Xdec = gtmp[:, 1024:2048].bitcast(BF16).rearrange("p (a b) -> p a b", a=32)
                ytok = AR.alloc([32, 64])
                ytmp = [AR.alloc([4, 64]) for _ in range(2)]
                gn_tok = AR.alloc([2048], BF16)
                rms = AR.alloc([24])
                B_dA, B_sm, B_cbm, B_X, B_ytok, B_gntok, B_rms = (
                    Buf(n) for n in ("dA", "sm", "cbm", "XG", "ytok", "gntok", "rms"))
                B_Xdec = B_X
                B_gtmp = B_X
                B_triDA = [Buf("triDA0"), Buf("triDA1"), Buf("triDA2")]
                B_LT = [Buf("LT0"), Buf("LT1")]
                B_scTt = [Buf("scT0"), Buf("scT1")]
                B_ytmp = [Buf("ytmp0"), Buf("ytmp1")]
                def trans(ci):
                    for grp8 in range(3):
                        ps, pb = next_ps()
                        psb = ps.bitcast(BF16)

                        def tr(h, psb=psb, grp8=grp8):
                            for q in range(8):
                                ch = grp8 * 8 + q
                                r = h.transpose(psb[:, q * 128:(q + 1) * 128], xbcT[:, ch, ci * 128:(ci + 1) * 128], identB)
                            return r
                        T.op("pe", tr, reads=[B_xsT if grp8 < 2 else B_BCT, B_identB], writes=[pb])
                        if grp8 < 2:
                            T.op("act", lambda h, psb=psb, grp8=grp8: h.copy(out=xs_tok[:, ci, grp8 * 1024:(grp8 + 1) * 1024], in_=psb),
                                 reads=[pb], writes=[B_xstoks[ci]])
                        else:
                            T.op("act", lambda h, psb=psb: h.copy(out=B_tok[:, ci, :], in_=psb), reads=[pb], writes=[B_Btoks[ci]])
                if cfg.get("stop") == "S_tr":
                    for ci in range(4):
                        trans(ci)
                else:
                    trans(0)
                if cfg.get("stop") == "S_tr":
                    tap("xstok", xs_tok, B_xstoks[3])
                    T.barrier()
                    return 'stop'
                gcount = 0
                for ci in range(4):
                    cs = slice(ci * 128, (ci + 1) * 128)
                    xs3 = xs_tok[:, ci, :].rearrange("p (a b) -> p a b", a=32)
                    T.op("dve", lambda h, ci=ci: h.tensor_tensor(out=dA, in0=dtt[:, ci, :], in1=negA, op=ALU.mult), reads=[B_dt, B_negA], writes=[B_dA])
                    ps, pb = next_ps()

                    def mmA(h, ps=ps):
                        h.matmul(ps[:, 0:32], triU, dA, start=True, stop=True)
                        return h.matmul(ps[:, 32:64], onesF, dA, start=True, stop=True)
                    T.op("pe", mmA, reads=[B_cst, B_dA], writes=[pb])
                    T.op("act", lambda h, ps=ps: h.copy(out=sm[:, 0, :], in_=ps[:, 0:32]), reads=[pb], writes=[B_sm])
                    T.op("act", lambda h, ps=ps: h.activation(out=sm[:, 1, :], in_=ps[:, 0:32], func=AF.Exp), reads=[pb], writes=[B_sm])
                    T.op("act", lambda h, ps=ps: h.activation(out=sm[:, 3, :], in_=ps[:, 32:64], func=AF.Exp), reads=[pb], writes=[B_sm])
                    T.op("dve", lambda h, ps=ps: h.tensor_tensor(out=sm[:, 2, :], in0=ps[:, 32:64], in1=sm[:, 0, :], op=ALU.subtract), reads=[pb, B_sm], writes=[B_sm])
                    T.op("act", lambda h: h.activation(out=sm[:, 2, :], in_=sm[:, 2, :], func=AF.Exp), reads=[B_sm], writes=[B_sm])
                    T.op("dve", lambda h, ci=ci: h.tensor_tensor(out=sm[:, 4, :], in0=sm[:, 2, :], in1=dtt[:, ci, :], op=ALU.mult), reads=[B_sm, B_dt], writes=[B_sm])
                    T.op("dve", lambda h, xs3=xs3, ci=ci: h.tensor_tensor(
                        out=Xb, in0=xs3, in1=dtt[:, ci, :].unsqueeze(2).to_broadcast([128, 32, 64]), op=ALU.mult), reads=[B_xstoks[ci], B_dt], writes=[B_X])
                    T.op("dve", lambda h, xs3=xs3: h.tensor_tensor(
                        out=Xdec, in0=xs3, in1=sm[:, 4, :].unsqueeze(2).to_broadcast([128, 32, 64]), op=ALU.mult), reads=[B_xstoks[ci], B_sm], writes=[B_Xdec])
                    if cfg.get("stop") == "S_pre":
                        tap("sm", sm, B_sm)
                        tap("Xb", Xb, B_X)
                        T.barrier()
                        return 'stop'
                    for half in range(2):
                        ps, pb = next_ps()

                        def mmcb(h, ps=ps, half=half, cs=cs):
                            for q in range(4):
                                g = half * 4 + q
                                r = h.matmul(ps[:, q * 128:(q + 1) * 128], xbcT[:, 16 + g, cs], xbcT[:, 24 + g, cs], start=True, stop=True)
                            return r
                        T.op("pe", mmcb, reads=[B_BCT], writes=[pb])
                        T.op("dve", lambda h, ps=ps, half=half: h.tensor_tensor(
                            out=cbm[:, half * 4:(half + 1) * 4, :], in0=ps.rearrange("p (a b) -> p a b", a=4),
                            in1=triU.unsqueeze(1).to_broadcast([128, 4, 128]), op=ALU.mult), reads=[pb, B_cst], writes=[B_cbm])
                    if cfg.get("stop") == "S_cb":
                        tap("cbm", cbm, B_cbm)
                        T.barrier()
                        return 'stop'
                    def front0(g):
                        s3 = g % 3
                        hs = slice(4 * g, 4 * g + 4)
                        T.op("pool", lambda h: h.tensor_tensor(
                            out=triDA[s3], in0=triU.unsqueeze(1).to_broadcast([128, 4, 128]),
                            in1=dA[:, hs].unsqueeze(2).to_broadcast([128, 4, 128]), op=ALU.mult), reads=[B_cst, B_dA], writes=[B_triDA[s3]])

                    def front1(g):
                        sl = g % 2
                        s3 = g % 3
                        ps_s, pb_s = next_ps()
                        T.op("pe", lambda h: h.matmul(ps_s, striL, triDA[s3].rearrange("p a b -> p (a b)"), start=True, stop=True),
                             reads=[B_cst, B_triDA[s3]], writes=[pb_s])
                        T.op("act", lambda h: h.activation(out=LT[sl].rearrange("p a b -> p (a b)"), in_=ps_s, func=AF.Exp),
                             reads=[pb_s], writes=[B_LT[sl]])

                    def front2(g):
                        sl = g % 2
                        T.op("dve", lambda h, sl=sl, g=g: h.tensor_tensor(
                            out=scTt[sl], in0=LT[sl], in1=cbm[:, g, :].unsqueeze(1).to_broadcast([128, 4, 128]), op=ALU.mult),
                            reads=[B_LT[sl], B_cbm], writes=[B_scTt[sl]])

                    def back(g, cs=cs, ci=ci):
                        sl = g % 2
                        hs = slice(4 * g, 4 * g + 4)
                        ps_y, pb_y = next_ps()

                        def mmy(h, ps_y=ps_y, g=g, sl=sl, hs=hs, cs=cs):
                            h.matmul(ps_y[:, 0:256], xbcT[:, 24 + g, cs], stbf[:, hs, :].rearrange("p a b -> p (a b)"), start=True, stop=True)
                            for hh in range(4):
                                r = h.matmul(ps_y[:, 256 + hh * 64:256 + (hh + 1) * 64], scTt[sl][:, hh, :], Xb[:, 4 * g + hh, :], start=True, stop=True)
                            return r
                        T.op("pe", mmy, reads=[B_BCT, B_stbf[g], B_scTt[sl], B_X], writes=[pb_y])
                        ps_n, pb_n = next_ps()
                        T.op("pe", lambda h, ps_n=ps_n, g=g, ci=ci, hs=hs: h.matmul(
                            ps_n[:, 0:256], B_tok[:, ci, g * 128:(g + 1) * 128], Xdec[:, hs, :].rearrange("p a b -> p (a b)"), start=True, stop=True),
                            reads=[B_Btoks[ci], B_Xdec], writes=[pb_n])
                        return sl, hs, ps_y, pb_y, ps_n, pb_n

                    def back2(g, sl, hs, ps_y, pb_y, ps_n, pb_n):
                        T.op("dve", lambda h: h.tensor_tensor(
                            out=ytmp[sl], in0=ps_y[:, 0:256].rearrange("p (a b) -> p a b", a=4),
                            in1=sm[:, 1, hs].unsqueeze(2).to_broadcast([128, 4, 64]), op=ALU.mult), reads=[pb_y, B_sm], writes=[B_ytmp[sl]])
                        T.op("dve", lambda h: h.tensor_tensor(
                            out=ytok[:, hs, :], in0=ps_y[:, 256:512].rearrange("p (a b) -> p a b", a=4), in1=ytmp[sl], op=ALU.add),
                            reads=[pb_y, B_ytmp[sl]], writes=[B_ytok])
                        T.op("dve", lambda h: h.tensor_tensor(
                            out=state[:, hs, :], in0=state[:, hs, :], in1=sm[:, 3, hs].unsqueeze(2).to_broadcast([128, 4, 64]), op=ALU.mult),
                            reads=[B_state[g], B_sm], writes=[B_state[g]])
                        T.op("dve", lambda h: h.tensor_tensor(
                            out=state[:, hs, :], in0=state[:, hs, :], in1=ps_n[:, 0:256].rearrange("p (a b) -> p a b", a=4), op=ALU.add),
                            reads=[B_state[g], pb_n], writes=[B_state[g]])
                        T.op("act", lambda h: h.copy(out=stbf[:, hs, :], in_=state[:, hs, :]), reads=[B_state[g]], writes=[B_stbf[g]])

                    front0(0)
                    front0(1)
                    front1(0)
                    front2(0)
                    for g in range(8):
                        if g + 2 < 8:
                            front0(g + 2)
                        if g + 1 < 8:
                            front1(g + 1)
                        bk = back(g)
                        if g + 1 < 8:
                            front2(g + 1)
                        back2(g, *bk)
                    if ci + 1 < 4:
                        trans(ci + 1)
                    if cfg.get("stop") == "S_grp":
                        tap("ytok", ytok, B_ytok)
                        T.barrier()
                        return 'stop'
                    g3 = gtmp.rearrange("p (a b) -> p a b", a=32)
                    T.op("dve", lambda h, xs3=xs3, g3=g3: h.tensor_tensor(
                        out=g3, in0=xs3, in1=rowv[:, R_D:R_D + 32].unsqueeze(2).to_broadcast([128, 32, 64]), op=ALU.mult),
                        reads=[B_xstoks[ci], B_rowv], writes=[B_gtmp])
                    T.op("dve", lambda h, g3=g3: h.tensor_tensor(out=ytok, in0=ytok, in1=g3, op=ALU.add), reads=[B_ytok, B_gtmp], writes=[B_ytok])
                    if t == 0 and ci == 1:
                        tap("y1", ytok, B_ytok)
                    T.op("dve", lambda h, ci=ci: h.tensor_tensor(out=gtmp, in0=ytok.rearrange("p a b -> p (a b)"), in1=sz[:, ci, :], op=ALU.mult),
                         reads=[B_ytok, B_sz], writes=[B_gtmp])
                    for g in range(8):
                        T.op("act", lambda h, g=g: h.activation(
                            out=ytok.rearrange("p a b -> p (a b)")[:, g * 256:(g + 1) * 256], in_=gtmp[:, g * 256:(g + 1) * 256],
                            func=AF.Square, accum_out=rms[:, g:g + 1]), reads=[B_gtmp], writes=[B_ytok, B_rms])
                    T.op("act", lambda h: h.activation(out=rms[:, 8:16], in_=rms[:, 0:8], func=AF.Sqrt, bias=epsrms, scale=1.0 / 256.0),
                         reads=[B_rms, B_cst], writes=[B_rms])
                    T.op("dve", lambda h: h.reciprocal(out=rms[:, 16:24], in_=rms[:, 8:16]), reads=[B_rms], writes=[B_rms])
                    for g in range(8):
                        T.op("dve", lambda h, g=g: h.scalar_tensor_tensor(
                            out=gn_tok[:, g * 256:(g + 1) * 256], in0=gtmp[:, g * 256:(g + 1) * 256], scalar=rms[:, 16 + g:17 + g],
                            in1=rowv[:, R_NW + g * 256:R_NW + (g + 1) * 256], op0=ALU.mult, op1=ALU.mult),
                            reads=[B_gtmp, B_rms, B_rowv], writes=[B_gntok])
                    if t == 0 and ci == 1:
                        tap("gn1", gn_tok, B_gntok)
                    if cfg.get("stop") == "S_gn":
                        tap("gntok", gn_tok, B_gntok)
                        T.barrier()
                        return 'stop'
                    for grp8 in range(2):
                        ps, pb = next_ps()
                        psb = ps.bitcast(BF16)

                        def tr2(h, psb=psb, grp8=grp8):
                            for q in range(8):
                                c0 = (grp8 * 8 + q) * 128
                                r = h.transpose(psb[:, q * 128:(q + 1) * 128], gn_tok[:, c0:c0 + 128], identB)
                            return r
                        T.op("pe", tr2, reads=[B_gntok, B_identB], writes=[pb])
                        T.op("act", lambda h, psb=psb, grp8=grp8, cs=cs: h.copy(
                            out=xbcT[:, grp8 * 8:(grp8 + 1) * 8, cs], in_=psb.rearrange("p (a b) -> p a b", a=8)),
                            reads=[pb], writes=[B_xsT])
                T.barrier()

            if stage_S() == 'stop':
                break
            def stage_O(t=t):
                AR.top = UNION_TOP
                gnT = xbcT
                sg = [AR.alloc([TT]) for _ in range(2)]
                m1 = [AR.alloc([TT]) for _ in range(4)]
                u = [AR.alloc([D]) for _ in range(2)]
                xt = [AR.alloc([D]) for _ in range(2)]
                xn = AR.alloc([D], BF16)
                st6 = AR.alloc([2, 6])
                mv = AR.alloc([8])
                h2t = [AR.alloc([8, 128], BF16) for _ in range(2)]
                st6a = [AR.alloc([2, 6]) for _ in range(2)]
                mva = [AR.alloc([8]) for _ in range(2)]
                B_smalla = [Buf("lnsmallA0"), Buf("lnsmallA1")]
                rt = AR.alloc([8, 64])
                m8 = AR.alloc([8, 8])
                rsm = AR.alloc([40])
                B_sg = [Buf("sg0"), Buf("sg1")]
                B_m1 = [Buf(f"m1{j}") for j in range(4)]
                B_u = [Buf("u0"), Buf("u1")]
                B_xt = [Buf("xt0"), Buf("xt1")]
                B_xn, B_small = Buf("xn"), Buf("lnsmall")
                B_h2t = [Buf("h2t0"), Buf("h2t1")]
                B_rt, B_m8, B_rsm = Buf("rt"), Buf("m8"), Buf("rsm")
                for half in range(2):
                    wga, bga = load_w(win_cols(C_GA + half * 512, 512), (8, 512))
                    for j in range(4):
                        ps, pb = next_ps()
                        proj_fm(wga, bga, j, ps, pb)
                        T.op("act", lambda h, ps=ps, j=j: h.activation(out=m1[j], in_=ps, func=AF.Sigmoid), reads=[pb], writes=[B_m1[j]])
                    wso = []
                    for pair in range(2):
                        c0 = half * 512 + pair * 256
                        wso.append(load_w(w_sso_d[:, c0:c0 + 256].rearrange("(kc p) n -> p kc n", p=128), (16, 256)))
                    for j in range(4):
                        wv, wb = wso[j // 2]
                        ps2, pb2 = next_ps()

                        def mma(h, ps2=ps2, wv=wv, j=j):
                            for kc in range(16):
                                r = h.matmul(ps2, wv[:, kc, (j % 2) * 128:(j % 2 + 1) * 128], gnT[:, kc, :], start=(kc == 0), stop=(kc == 15))
                            return r
                        T.op("pe", mma, reads=[wb, B_xsT], writes=[pb2])
                        T.op("dve", lambda h, ps2=ps2, j=j: h.tensor_tensor(out=m1[j], in0=ps2, in1=m1[j], op=ALU.mult),
                             reads=[pb2, B_m1[j]], writes=[B_m1[j]])
                    wgb, bgb = load_w(win_cols(C_GB + half * 512, 512), (8, 512))
                    wsc, bsc = load_w(w_sco_d[:, half * 512:(half + 1) * 512].rearrange("(kc p) n -> p kc n", p=128), (8, 512))
                    for j in range(4):
                        dj = half * 4 + j
                        sl = j % 2
                        ps3, pb3 = next_ps()
                        proj_fm(wgb, bgb, j, ps3, pb3)
                        T.op("act", lambda h, ps3=ps3, sl=sl: h.activation(out=sg[sl], in_=ps3, func=AF.Sigmoid), reads=[pb3], writes=[B_sg[sl]])
                        ps4, pb4 = next_ps()

                        def mmb(h, ps4=ps4, wsc=wsc, j=j):
                            for kc in range(8):
                                r = h.matmul(ps4, wsc[:, kc, j * 128:(j + 1) * 128], ubT[:, kc, :], start=(kc == 0), stop=(kc == 7))
                            return r
                        T.op("pe", mmb, reads=[bsc, B_ubT], writes=[pb4])
                        T.op("dve", lambda h, ps4=ps4, sl=sl: h.tensor_tensor(out=sg[sl], in0=ps4, in1=sg[sl], op=ALU.mult),
                             reads=[pb4, B_sg[sl]], writes=[B_sg[sl]])
                        T.op("dve", lambda h, sl=sl, dj=dj, j=j: h.tensor_tensor(out=mergedT[:, dj, :], in0=m1[j], in1=sg[sl], op=ALU.add),
                             reads=[B_m1[j], B_sg[sl]], writes=B_Btoks)
                if t == 0:
                    tap("mergedT", mergedT, B_Btoks[3])
                wo0, bo0 = load_w(w_o_d[:, 0:512].rearrange("(kc p) n -> p kc n", p=128), (8, 512))
                wo1, bo1 = load_w(w_o_d[:, 512:1024].rearrange("(kc p) n -> p kc n", p=128), (8, 512))

                def partA(ci):
                    chunk = t * 4 + ci
                    tok0 = chunk * 128
                    sl = ci % 2
                    T.dma("sp", [lambda h: h.dma_start(out=xt[sl], in_=x_d[tok0:tok0 + 128, :])], x_sem[sl], writes=[B_xt[sl]])
                    yield
                    for hf, (wo, bo) in enumerate(((wo0, bo0), (wo1, bo1))):
                        ps, pb = next_ps()

                        def mmo(h, ps=ps, wo=wo):
                            for kc in range(8):
                                r = h.matmul(ps, mergedT[:, kc, ci * 128:(ci + 1) * 128], wo[:, kc, :], start=(kc == 0), stop=(kc == 7))
                            return r
                        T.op("pe", mmo, reads=[bo] + B_Btoks, writes=[pb])
                        yield
                        T.op("dve", lambda h, ps=ps, hf=hf: h.tensor_tensor(
                            out=u[sl][:, hf * 512:(hf + 1) * 512], in0=ps, in1=g1b[:, hf * 512:(hf + 1) * 512], op=ALU.mult),
                            reads=[pb, B_g1b], writes=[B_u[sl]])
                        yield
                    if t == 0 and ci == 1:
                        tap("gmix1", u[sl], B_u[sl])
                    T.op("dve", lambda h: h.scalar_tensor_tensor(out=u[sl], in0=xt[sl], scalar=float(ALPHA), in1=u[sl], op0=ALU.mult, op1=ALU.add),
                         reads=[B_xt[sl], B_u[sl]], writes=[B_u[sl]])
                    yield
                    yield from ln_stats_g(u[sl], B_u[sl], st6a[sl], mva[sl], mva[sl][:, 3:4], B_smalla[sl])
                    T.op("dve", lambda h: h.tensor_scalar(out=u[sl], in0=u[sl], scalar1=mva[sl][:, 0:1], scalar2=mva[sl][:, 3:4], op0=ALU.subtract, op1=ALU.mult),
                         reads=[B_u[sl], B_smalla[sl]], writes=[B_u[sl]])
                    yield
                    T.op("dve", lambda h: h.tensor_tensor(out=u[sl], in0=u[sl], in1=rowv[:, R_L1G:R_L1G + D], op=ALU.mult), reads=[B_u[sl], B_rowv], writes=[B_u[sl]])
                    yield
                    T.op("dve", lambda h: h.tensor_tensor(out=u[sl], in0=u[sl], in1=rowv[:, R_L1B:R_L1B + D], op=ALU.add), reads=[B_u[sl], B_rowv], writes=[B_u[sl]])
                    yield
                    T.dma("sp", [lambda h: h.dma_start(out=x1_d[tok0:tok0 + 128, :], in_=u[sl])], x1st_sem[sl], reads=[B_u[sl]])
                    yield
                    if t == 0 and ci == 1:
                        tap("x1_1", u[sl], B_u[sl])

                def partB(ci):
                    chunk = t * 4 + ci
                    tok0 = chunk * 128
                    sl = ci % 2
                    yield from ln_mod_T_g(u[sl], B_u[sl], xn, B_xn, st6, mv, B_small, 16, lambda kc: h2t[sl][:, kc, :], B_h2t[sl])
                    T.dma("sp", [lambda h: h.dma_start(out=h2T_d[:, :, tok0:tok0 + 128], in_=h2t[sl])], h2st_sem[sl], reads=[B_h2t[sl]])
                    yield
                    if t == 0 and ci == 1:
                        tap("h2t1", h2t[sl], B_h2t[sl])
                    ps, pb = next_ps()

                    def mmr(h):
                        for kc in range(8):
                            r = h.matmul(ps[:, 0:64], h2t[sl][:, kc, :], rw[:, kc, :], start=(kc == 0), stop=(kc == 7))
                        return r
                    T.op("pe", mmr, reads=[B_h2t[sl], B_rw], writes=[pb])
                    yield
                    sc_, bi_, mk_, se_ = rt[:, 0, :], rt[:, 1, :], rt[:, 2, :], rt[:, 3, :]
                    T.op("act", lambda h: h.activation(out=sc_, in_=ps[:, 0:64], func=AF.Sigmoid), reads=[pb], writes=[B_rt])
                    yield
                    T.op("dve", lambda h: h.tensor_tensor(out=bi_, in0=sc_, in1=rowv[:, R_RB:R_RB + 64], op=ALU.add), reads=[B_rt, B_rowv], writes=[B_rt])
                    yield
                    for g in range(8):
                        T.op("dve", lambda h, g=g: h.max(out=m8[:, g, :], in_=bi_[:, g * 8:(g + 1) * 8]), reads=[B_rt], writes=[B_m8])
                        yield
                    T.op("dve", lambda h: h.tensor_tensor(out=rsm[:, 0:8], in0=m8[:, :, 0], in1=m8[:, :, 1], op=ALU.add), reads=[B_m8], writes=[B_rsm])
                    yield
                    T.op("dve", lambda h: h.max(out=rsm[:, 8:16], in_=rsm[:, 0:8]), reads=[B_rsm], writes=[B_rsm])
                    yield
                    T.op("dve", lambda h: h.tensor_scalar(out=rsm[:, 16:24], in0=rsm[:, 0:8], scalar1=rsm[:, 11:12], scalar2=None, op0=ALU.is_ge),
                         reads=[B_rsm], writes=[B_rsm])
                    yield
                    T.op("dve", lambda h: h.tensor_scalar(out=rsm[:, 24:32], in0=rsm[:, 16:24], scalar1=-1.0, scalar2=1.0e9, op0=ALU.add, op1=ALU.mult),
                         reads=[B_rsm], writes=[B_rsm])
                    yield
                    mk3 = mk_.rearrange("p (a b) -> p a b", a=8)
                    T.op("dve", lambda h: h.tensor_tensor(
                        out=mk3, in0=bi_.rearrange("p (a b) -> p a b", a=8), in1=rsm[:, 16:24].unsqueeze(2).to_broadcast([128, 8, 8]), op=ALU.mult),
                        reads=[B_rt, B_rsm], writes=[B_rt])
                    yield
                    T.op("dve", lambda h: h.tensor_tensor(
                        out=mk3, in0=mk3, in1=rsm[:, 24:32].unsqueeze(2).to_broadcast([128, 8, 8]), op=ALU.add), reads=[B_rt, B_rsm], writes=[B_rt])
                    yield
                    T.op("dve", lambda h: h.max(out=rsm[:, 8:16], in_=mk_), reads=[B_rt], writes=[B_rsm])
                    yield
                    T.op("dve", lambda h: h.tensor_scalar(out=se_, in0=mk_, scalar1=rsm[:, 15:16], scalar2=None, op0=ALU.is_ge), reads=[B_rt, B_rsm], writes=[B_rt])
                    yield
                    T.op("dve", lambda h: h.tensor_tensor(out=se_, in0=se_, in1=sc_, op=ALU.mult), reads=[B_rt], writes=[B_rt])
                    yield
                    T.op("dve", lambda h: h.tensor_reduce(out=rsm[:, 32:33], in_=se_, axis=AX.X, op=ALU.add), reads=[B_rt], writes=[B_rsm])
                    yield
                    T.op("dve", lambda h: h.reciprocal(out=rsm[:, 33:34], in_=rsm[:, 32:33]), reads=[B_rsm], writes=[B_rsm])
                    yield
                    T.op("dve", lambda h: h.tensor_scalar(
                        out=G_all[:, chunk, 0:64], in0=se_, scalar1=rsm[:, 33:34], scalar2=2.5, op0=ALU.mult, op1=ALU.mult),
                        reads=[B_rt, B_rsm], writes=[B_G])
                    yield

                for step in range(5):
                    run_interleaved([partA(step) if step < 4 else None, partB(step - 1) if step >= 1 else None])
                T.barrier()
            if stage_O() == 'stop':
                break
        tap("G", G_all, B_G)

        def phase_B():
            T.barrier(final=True)
            AR.top = PERSIST_TOP
            NB = 2 if n_tiles == NTILE else 1
            BT = S // 2 if n_tiles == NTILE else n_tiles * TT
            nsub = BT // 128
            n512 = BT // 512
            h2blk = AR.alloc([8, BT], BF16)
            acc = AR.alloc([nsub, D])
            wgu = [AR.alloc([8, 512], BF16) for _ in range(2)]
            wdn = [AR.alloc([2, D], BF16) for _ in range(2)]
            sgt = [AR.alloc([512]) for _ in range(2)]
            actT = [AR.alloc([2, 512], BF16) for _ in range(2)]
            xt = [AR.alloc([D]) for _ in range(2)]
            st6 = AR.alloc([2, 6])
            mv = AR.alloc([8])
            B_h2blk, B_small = Buf("h2blk"), Buf("lnsmallB")
            B_acc = [Buf(f"acc{i}") for i in range(nsub)]
            B_wgu = [Buf("wgu0"), Buf("wgu1")]
            B_wdn = [Buf("wdn0"), Buf("wdn1")]
            B_sgt = [Buf("sgt0"), Buf("sgt1")]
            B_actT = [Buf("actT0"), Buf("actT1")]
            B_xt = [Buf("xtB0"), Buf("xtB1")]
            wgu_sem = [new_dsem("wgu0"), new_dsem("wgu1")]
            wdn_sem = [new_dsem("wdn0"), new_dsem("wdn1")]
            h2_sem = new_dsem("h2")
            out_sems = [new_dsem(f"out{i}") for i in range(4)]
            B_out = Buf("out")
            for blk in range(NB):
                t0 = blk * BT
                T.dma("sp", [lambda h, t0=t0: h.dma_start(out=h2blk, in_=h2T_d[:, :, t0:t0 + BT])], h2_sem, writes=[B_h2blk])
                T.op("dve", lambda h: h.memset(acc, 0.0), writes=B_acc)
                aslot = 0
                for e in range(n_exp):
                    ee = e if n_exp == NE else (e if e < n_exp - 1 else NE - 1)
                    sl = e % 2
                    T.dma("pool", [
                        lambda h, sl=sl, ee=ee: h.dma_start(out=wgu[sl][:, :, 0:256], in_=wg_d[ee].rearrange("(kc p) n -> p kc n", p=128)),
                        lambda h, sl=sl, ee=ee: h.dma_start(out=wgu[sl][:, :, 256:512], in_=wu_d[ee].rearrange("(kc p) n -> p kc n", p=128)),
                    ], wgu_sem[sl], writes=[B_wgu[sl]])
                    T.dma("pool", [lambda h, sl=sl, ee=ee: h.dma_start(out=wdn[sl], in_=wd_d[ee].rearrange("(fc p) n -> p fc n", p=128))],
                          wdn_sem[sl], writes=[B_wdn[sl]])
                    for t4 in range(n512):
                        ts_ = slice(t4 * 512, (t4 + 1) * 512)
                        asl = aslot % 2
                        aslot += 1
                        for fc in range(2):
                            ssl = fc
                            ps_g, pb_g = next_ps()
                            ps_u, pb_u = next_ps()

                            def mmg(h, ps_g=ps_g, sl=sl, fc=fc, ts_=ts_):
                                for kc in range(8):
                                    r = h.matmul(ps_g, wgu[sl][:, kc, fc * 128:(fc + 1) * 128], h2blk[:, kc, ts_], start=(kc == 0), stop=(kc == 7))
                                return r

                            def mmu(h, ps_u=ps_u, sl=sl, fc=fc, ts_=ts_):
                                for kc in range(8):
                                    r = h.matmul(ps_u, wgu[sl][:, kc, 256 + fc * 128:256 + (fc + 1) * 128], h2blk[:, kc, ts_], start=(kc == 0), stop=(kc == 7))
                                return r
                            T.op("pe", mmg, reads=[B_wgu[sl], B_h2blk], writes=[pb_g])
                            T.op("pe", mmu, reads=[B_wgu[sl], B_h2blk], writes=[pb_u])
                            T.op("act", lambda h, ps_g=ps_g, ssl=ssl: h.activation(out=sgt[ssl], in_=ps_g, func=AF.Silu), reads=[pb_g], writes=[B_sgt[ssl]])
                            T.op("dve", lambda h, ps_u=ps_u, ssl=ssl, asl=asl, fc=fc: h.tensor_tensor(
                                out=actT[asl][:, fc, :], in0=ps_u, in1=sgt[ssl], op=ALU.mult), reads=[pb_u, B_sgt[ssl]], writes=[B_actT[asl]])
                        for sub in range(4):
                            ti = t4 * 4 + sub
                            gcol = G_all[:, blk * nsub + ti, ee:ee + 1]
                            for hf in range(2):
                                ps_d, pb_d = next_ps()

                                def mmd(h, ps_d=ps_d, asl=asl, sub=sub, sl=sl, hf=hf):
                                    for fc in range(2):
                                        r = h.matmul(ps_d, actT[asl][:, fc, sub * 128:(sub + 1) * 128], wdn[sl][:, fc, hf * 512:(hf + 1) * 512],
                                                     start=(fc == 0), stop=(fc == 1))
                                    return r
                                T.op("pe", mmd, reads=[B_actT[asl], B_wdn[sl]], writes=[pb_d])
                                T.op("dve", lambda h, ps_d=ps_d, ti=ti, hf=hf, gcol=gcol: h.scalar_tensor_tensor(
                                    out=acc[:, ti, hf * 512:(hf + 1) * 512], in0=ps_d, scalar=gcol, in1=acc[:, ti, hf * 512:(hf + 1) * 512],
                                    op0=ALU.mult, op1=ALU.add), reads=[pb_d, B_G, B_acc[ti]], writes=[B_acc[ti]])
                if blk == 0:
                    tap("ffn1", acc[:, 1, :], B_acc[1])
                for ti in range(nsub):
                    tok0 = t0 + ti * 128
                    sl = ti % 2
                    T.dma("sp", [lambda h, sl=sl, tok0=tok0: h.dma_start(out=xt[sl], in_=x1_d[tok0:tok0 + 128, :])], x_sem[sl], writes=[B_xt[sl]])
                    a = acc[:, ti, :]
                    T.op("dve", lambda h, a=a: h.tensor_tensor(out=a, in0=a, in1=g2b, op=ALU.mult), reads=[B_acc[ti], B_g2b], writes=[B_acc[ti]])
                    T.op("dve", lambda h, a=a, sl=sl: h.scalar_tensor_tensor(out=a, in0=xt[sl], scalar=float(ALPHA), in1=a, op0=ALU.mult, op1=ALU.add),
                         reads=[B_xt[sl], B_acc[ti]], writes=[B_acc[ti]])
                    ln_stats(a, B_acc[ti], st6, mv, mv[:, 3:4], B_small)
                    T.op("dve", lambda h, a=a: h.tensor_scalar(out=a, in0=a, scalar1=mv[:, 0:1], scalar2=mv[:, 3:4], op0=ALU.subtract, op1=ALU.mult),
                         reads=[B_acc[ti], B_small], writes=[B_acc[ti]])
                    T.op("dve", lambda h, a=a: h.tensor_tensor(out=a, in0=a, in1=rowv[:, R_L2G:R_L2G + D], op=ALU.mult), reads=[B_acc[ti], B_rowv], writes=[B_acc[ti]])
                    T.op("dve", lambda h, a=a: h.tensor_tensor(out=a, in0=a, in1=rowv[:, R_L2B:R_L2B + D], op=ALU.add), reads=[B_acc[ti], B_rowv], writes=[B_acc[ti]])
                    T.dma("sp", [lambda h, a=a, tok0=tok0: h.dma_start(out=out_d[tok0:tok0 + 128, :], in_=a)], out_sems[ti % 4], reads=[B_acc[ti]], writes=[B_out])
                T.barrier()
        if do_moe:
            phase_B()
        T.barrier(final=True)
        with nc.Block() as block:
            T.emit(block)
    return nc, tap_out


def _host_pack(inputs, b):
    f = np.float32
    g = lambda k: np.asarray(inputs[k], dtype=f)
    consts = np.zeros((128, NCONST), f)
    consts[:, K_ID:K_ID + 128] = np.eye(128, dtype=f)
    jj, ll = np.meshgrid(np.arange(128), np.arange(128), indexing="ij")
    consts[:, K_TRIU:K_TRIU + 128] = (jj <= ll)
    consts[:, K_STRIL:K_STRIL + 128] = (jj > ll)
    consts[:, K_ONES:K_ONES + 128] = 1.0
    consts[:, K_EPSLN] = LN_EPS
    consts[:, K_EPSRMS] = RMS_EPS
    consts[:, K_ONE] = 1.0
    rowv = np.concatenate([g("ssd_dt_bias")[0], g("ssd_A_log")[0], g("ssd_D")[0], g("router_bias")[0], g("ssd_norm_w")[0],
                           g("ln1_g")[0], g("ln1_b")[0], g("ln2_g")[0], g("ln2_b")[0]])[None, :]
    assert rowv.shape[1] == NROW
    cw = g("ssd_conv_w")[0].reshape(4, 32, 128).transpose(2, 1, 0).reshape(128, 128)
    cb = g("ssd_conv_b")[0].reshape(32, 128).T
    scw = g("sc_conv_w")[0].reshape(3, 8, 128).transpose(2, 1, 0).reshape(128, 24)
    colv = np.ascontiguousarray(np.concatenate([cw, cb, scw], axis=1))
    assert colv.shape[1] == NCOL
    m = {
        "x": np.ascontiguousarray(g("x")[b]),
        "cT": np.ascontiguousarray(g("c")[b].reshape(8, 128).T),
        "w_ada": g("w_ada")[0], "b_ada": g("b_ada")[0][None, :], "w_in": g("w_in")[0],
        "colv": colv, "rowv": np.ascontiguousarray(rowv), "consts": consts,
        "w_ssd_out": g("w_ssd_out")[0], "w_sc_out": g("w_sc_out")[0], "w_o": g("w_o")[0], "router_w": g("router_w")[0],
    }
    return m


_SHARED = {}


def _shared_pack(inputs):
    f = np.float32
    g = lambda k: np.asarray(inputs[k], dtype=f)
    return {
        "w_gate": np.concatenate([g("w_gate")[0], g("sh_gate")], axis=0),
        "w_up": np.concatenate([g("w_up")[0], g("sh_up")], axis=0),
        "w_down": np.concatenate([g("w_down")[0], g("sh_down")], axis=0),
    }


def kernel(**inputs):
    nc, _ = build()
    shared = _shared_pack(inputs)
    in_maps = []
    for b in range(8):
        m = _host_pack(inputs, b)
        m.update(shared)
        in_maps.append(m)
    res = run_bass_kernel_spmd(nc, in_maps, core_ids=list(range(8)))
    return np.stack([np.asarray(r["out"], dtype=np.float32) for r in res.results], axis=0)
```

```python
import numpy as np
from contextlib import ExitStack
import concourse.bass as bass
import concourse.mybir as mybir
from concourse.bass_utils import run_bass_kernel_spmd

F32 = mybir.dt.float32
BF16 = mybir.dt.bfloat16
AF = mybir.ActivationFunctionType
ALU = mybir.AluOpType
AX = mybir.AxisListType

S = 4096
D = 1024
TT = 512
NTILE = S // TT
NE = 65
ALPHA = 2.0 ** 0.25
LN_EPS = 1e-5
RMS_EPS = 1e-5
INCOLS = 11296
C_Z, C_XBC, C_DT, C_SCB, C_SCC, C_SCH, C_GA, C_GB = 0, 2048, 6144, 6176, 7200, 8224, 9248, 10272
R_DTB, R_ALOG, R_D, R_RB, R_NW, R_L1G, R_L1B, R_L2G, R_L2B = 0, 32, 64, 96, 160, 2208, 3232, 4256, 5280
NROW = 6304
CV_CW, CV_CB, CV_SCW = 0, 128, 160
NCOL = 184
K_ID, K_TRIU, K_STRIL, K_ONES, K_EPSLN, K_EPSRMS, K_ONE = 0, 128, 256, 384, 512, 513, 514
NCONST = 516
ARENA_WORDS = 52600


class Buf:
    __slots__ = ("name", "w", "r")

    def __init__(self, name):
        self.name = name
        self.w = None
        self.r = {}


class Trk:
    ENGS = ("pe", "act", "dve", "pool", "sp")

    def __init__(self, nc, stack):
        self.nc = nc
        self.stack = stack
        self.sem = {}
        self.cnt = {}
        self.q = {e: [] for e in self.ENGS}
        self.seen = {e: {} for e in self.ENGS}
        for e in self.ENGS:
            self.newsem("E_" + e)

    def newsem(self, name):
        self.sem[name] = self.stack.enter_context(self.nc.semaphore(name))
        self.cnt[name] = 0
        return name

    def _waits(self, eng, reads, writes):
        need = {}
        own = "E_" + eng

        def add(s, v):
            if v > need.get(s, 0):
                need[s] = v
        for b in reads:
            if b.w:
                add(*b.w)
        for b in writes:
            if b.w:
                add(*b.w)
            for s, v in b.r.items():
                if s != own:
                    add(s, v)
        out = []
        for s, v in need.items():
            if s == own and eng == "pe":
                continue
            if self.seen[eng].get(s, 0) >= v:
                continue
            self.seen[eng][s] = v
            out.append((s, v))
        return out

    def op(self, eng, fn, reads=(), writes=()):
        w = self._waits(eng, reads, writes)
        s = "E_" + eng
        self.cnt[s] += 1
        v = self.cnt[s]
        self.q[eng].append((w, fn, s, 1))
        for b in reads:
            b.r[s] = v
        for b in writes:
            b.w = (s, v)
            b.r = {}

    def dma(self, eng, fns, dsem, reads=(), writes=()):
        w = self._waits(eng, reads, writes)
        for i, fn in enumerate(fns):
            self.q[eng].append((w if i == 0 else [], fn, dsem, 16))
        self.cnt[dsem] += 16 * len(fns)
        v = self.cnt[dsem]
        for b in reads:
            b.r[dsem] = v
        for b in writes:
            b.w = (dsem, v)
            b.r = {}

    def barrier(self, final=False):
        snap = dict(self.cnt)
        for e in self.ENGS:
            if e == "pool" and not final:
                continue
            w = []
            for s, v in snap.items():
                if v > 0 and self.seen[e].get(s, 0) < v and not (s == "E_" + e and e == "pe"):
                    self.seen[e][s] = v
                    w.append((s, v))
            if w:
                self.q[e].append((w, None, None, 0))

    def emit(self, block):
        sem = self.sem

        def run(h, q):
            for waits, fn, s, inc in q:
                for ws, wv in waits:
                    h.wait_ge(sem[ws], wv)
                if fn is not None:
                    fn(h).then_inc(sem[s], inc)

        @block.tensor
        def _(h):
            run(h, self.q["pe"])

        @block.scalar
        def _(h):
            run(h, self.q["act"])

        @block.vector
        def _(h):
            run(h, self.q["dve"])

        @block.gpsimd
        def _(h):
            run(h, self.q["pool"])

        @block.sync
        def _(h):
            run(h, self.q["sp"])


class Arena:
    def __init__(self, ap, nwords):
        self.ap = ap
        self.n = nwords
        self.top = 0

    def alloc(self, shape, dtype=F32):
        n = int(np.prod(shape))
        words = n if dtype == F32 else (n + 1) // 2
        off = self.top
        self.top += words
        assert self.top <= self.n, ("arena overflow", self.top, self.n)
        v = self.ap[:, off:off + words]
        if dtype == BF16:
            v = v.bitcast(BF16)
        if len(shape) == 2:
            v = v.rearrange("p (a b) -> p a b", a=shape[0])
        elif len(shape) == 3:
            v = v.rearrange("p (a b c) -> p a b c", a=shape[0], b=shape[1])
        return v


def build(cfg=None):
    cfg = cfg or {}
    n_tiles = cfg.get("n_tiles", NTILE)
    do_moe = cfg.get("moe", True)
    n_exp = cfg.get("n_exp", NE)
    taps = cfg.get("taps", ())
    nc = bass.Bass("TRN2", target_bir_lowering=False)

    def din(name, shape, dt=F32):
        return nc.dram_tensor(name, list(shape), dt, kind="ExternalInput").ap()

    x_d = din("x", [S, D])
    cT_d = din("cT", [128, 8])
    w_ada_d = din("w_ada", [D, 6 * D])
    b_ada_d = din("b_ada", [1, 6 * D])
    w_in_d = din("w_in", [D, INCOLS])
    colv_d = din("colv", [128, NCOL])
    rowv_d = din("rowv", [1, NROW])
    cst_d = din("consts", [128, NCONST])
    w_sso_d = din("w_ssd_out", [2048, D])
    w_sco_d = din("w_sc_out", [D, D])
    w_o_d = din("w_o", [D, D])
    rw_d = din("router_w", [D, 64])
    wg_d = din("w_gate", [NE, D, 256])
    wu_d = din("w_up", [NE, D, 256])
    wd_d = din("w_down", [NE, 256, D])
    out_d = nc.dram_tensor("out", [S, D], F32, kind="ExternalOutput").ap()
    x1_d = nc.dram_tensor("x1_scr", [S, D], F32, kind="Internal").ap()
    h2T_d = nc.dram_tensor("h2T_scr", [128, 8, S], BF16, kind="Internal").ap()

    tap_out = {}
    with ExitStack() as stack:
        arena_t = stack.enter_context(nc.sbuf_tensor("arena", [128, ARENA_WORDS], F32))
        ps_t = [stack.enter_context(nc.psum_tensor(f"ps{i}", [128, 512], F32)) for i in range(8)]
        T = Trk(nc, stack)
        AR = Arena(arena_t[:], ARENA_WORDS)
        PSB = [Buf(f"ps{i}") for i in range(8)]
        ps_rr = [0]

        def next_ps():
            i = ps_rr[0] % 8
            ps_rr[0] += 1
            return ps_t[i][:], PSB[i]

        dsem_n = [0]

        def new_dsem(tag):
            dsem_n[0] += 1
            return T.newsem(f"D{dsem_n[0]}_{tag}")

        tap_sem = new_dsem("tap")

        def tap(name, ap, buf):
            if name not in taps:
                return
            dt = ap.dtype
            t = nc.dram_tensor("tap_" + name, list(ap.shape), dt, kind="ExternalOutput").ap()
            tap_out[name] = t
            T.dma("sp", [lambda h, t=t, ap=ap: h.dma_start(out=t, in_=ap)], tap_sem, reads=[buf], writes=[])

        cst = AR.alloc([NCONST])
        identF = cst[:, K_ID:K_ID + 128]
        triU = cst[:, K_TRIU:K_TRIU + 128]
        striL = cst[:, K_STRIL:K_STRIL + 128]
        onesF = cst[:, K_ONES:K_ONES + 128]
        epsln = cst[:, K_EPSLN:K_EPSLN + 1]
        epsrms = cst[:, K_EPSRMS:K_EPSRMS + 1]
        onecol = cst[:, K_ONE:K_ONE + 1]
        identB = AR.alloc([128], BF16)
        rowv = AR.alloc([NROW])
        colv = AR.alloc([NCOL])
        modcol = AR.alloc([32])
        g1b = AR.alloc([D])
        g2b = AR.alloc([D])
        negA = AR.alloc([32])
        G_all = AR.alloc([32, NE])
        rw = AR.alloc([8, 64], BF16)
        halo = AR.alloc([32, 3])
        halo2 = AR.alloc([8, 2])
        state = AR.alloc([32, 64])
        stbf = AR.alloc([32, 64], BF16)
        B_cst, B_identB, B_rowv, B_colv, B_modcol, B_g1b, B_g2b, B_negA, B_G, B_rw = (
            Buf(n) for n in ("cst", "identB", "rowv", "colv", "modcol", "g1b", "g2b", "negA", "G", "rw"))
        B_halo = [Buf(f"halo{i}") for i in range(32)]
        B_halo2 = [Buf(f"halo2_{i}") for i in range(8)]
        B_state = [Buf(f"state{g}") for g in range(8)]
        B_stbf = [Buf(f"stbf{g}") for g in range(8)]
        PERSIST_TOP = AR.top

        ld_sem = new_dsem("ld")
        T.dma("sp", [lambda h: h.dma_start(out=cst, in_=cst_d[:, :])], new_dsem("cst"), writes=[B_cst])
        T.dma("sp", [lambda h: h.dma_start(out=rowv, in_=rowv_d[0, :].partition_broadcast(128))], new_dsem("rowv"), writes=[B_rowv])
        T.dma("sp", [lambda h: h.dma_start(out=colv, in_=colv_d[:, :])], new_dsem("colv"), writes=[B_colv])
        rw_sem = new_dsem("rw")
        T.dma("pool", [lambda h: h.dma_start(out=rw, in_=rw_d.rearrange("(kc p) n -> p kc n", p=128))], rw_sem, writes=[B_rw])
        T.op("dve", lambda h: h.tensor_copy(out=identB, in_=identF), reads=[B_cst], writes=[B_identB])
        T.op("act", lambda h: h.activation(out=negA, in_=rowv[:, R_ALOG:R_ALOG + 32], func=AF.Exp), reads=[B_rowv], writes=[B_negA])
        T.op("dve", lambda h: h.tensor_scalar(out=negA, in0=negA, scalar1=-1.0, scalar2=None, op0=ALU.mult), reads=[B_negA], writes=[B_negA])
        T.op("dve", lambda h: h.memset(halo, 0.0), writes=B_halo)
        T.op("dve", lambda h: h.memset(halo2, 0.0), writes=B_halo2)
        T.op("dve", lambda h: h.memset(state, 0.0), writes=B_state)
        T.op("dve", lambda h: h.memset(stbf, 0.0), writes=B_stbf)
        T.op("dve", lambda h: h.memset(G_all, 1.0), writes=[B_G])

        cTt = AR.alloc([8])
        scT = AR.alloc([8])
        modrow = AR.alloc([6 * D])
        badar = AR.alloc([6 * D])
        wa = [AR.alloc([8, 512]) for _ in range(2)]
        B_cT, B_scT, B_modrow, B_badar = Buf("cT"), Buf("scT"), Buf("modrow"), Buf("badar")
        B_wa = [Buf("wa0"), Buf("wa1")]
        wa_sem = [new_dsem("wa0"), new_dsem("wa1")]
        T.dma("sp", [lambda h: h.dma_start(out=cTt, in_=cT_d[:, :])], new_dsem("cT"), writes=[B_cT])
        T.dma("sp", [lambda h: h.dma_start(out=badar[0:1, :], in_=b_ada_d[:, :])], new_dsem("bada"), writes=[B_badar])
        T.op("act", lambda h: h.activation(out=scT, in_=cTt, func=AF.Silu), reads=[B_cT], writes=[B_scT])
        for nb in range(12):
            sl = nb % 2
            T.dma("sp", [lambda h, nb=nb, sl=sl: h.dma_start(
                out=wa[sl], in_=w_ada_d[:, nb * 512:(nb + 1) * 512].rearrange("(kc p) n -> p kc n", p=128))],
                wa_sem[sl], writes=[B_wa[sl]])
            ps, pb = next_ps()

            def mm(h, ps=ps, sl=sl):
                for kc in range(8):
                    r = h.matmul(ps[0:1, :], scT[:, kc:kc + 1], wa[sl][:, kc, :], start=(kc == 0), stop=(kc == 7))
                return r
            T.op("pe", mm, reads=[B_scT, B_wa[sl]], writes=[pb])
            T.op("dve", lambda h, ps=ps, nb=nb: h.tensor_tensor(
                out=modrow[0:1, nb * 512:(nb + 1) * 512], in0=ps[0:1, :], in1=badar[0:1, nb * 512:(nb + 1) * 512], op=ALU.add),
                reads=[pb, B_badar], writes=[B_modrow])
        ps, pb = next_ps()

        def mmcol(h, ps=ps):
            for j in range(32):
                off = [0, 1024, 3072, 4096][j // 8] + (j % 8) * 128
                r = h.matmul(ps[:, j:j + 1], modrow[0:1, off:off + 128], onecol[0:1, 0:1], start=True, stop=True)
            return r
        T.op("pe", mmcol, reads=[B_modrow, B_cst], writes=[pb])
        T.op("dve", lambda h, ps=ps: h.tensor_copy(out=modcol, in_=ps[:, 0:32]), reads=[pb], writes=[B_modcol])
        T.op("dve", lambda h: h.tensor_scalar(out=modcol[:, 8:16], in0=modcol[:, 8:16], scalar1=1.0, scalar2=None, op0=ALU.add),
             reads=[B_modcol], writes=[B_modcol])
        T.op("dve", lambda h: h.tensor_scalar(out=modcol[:, 24:32], in0=modcol[:, 24:32], scalar1=1.0, scalar2=None, op0=ALU.add),
             reads=[B_modcol], writes=[B_modcol])
        for gi, (gb_, bb) in enumerate(((g1b, B_g1b), (g2b, B_g2b))):
            for hf in range(2):
                ps, pb = next_ps()
                off = (2048 if gi == 0 else 5120) + hf * 512
                T.op("pe", lambda h, ps=ps, off=off: h.matmul(ps, onesF[0:1, :], modrow[0:1, off:off + 512], start=True, stop=True),
                     reads=[B_modrow, B_cst], writes=[pb])
                T.op("act", lambda h, ps=ps, gb_=gb_, hf=hf: h.copy(out=gb_[:, hf * 512:(hf + 1) * 512], in_=ps),
                     reads=[pb], writes=[bb])
        tap("modrow", modrow[0:1, :], B_modrow)
        tap("modcol", modcol, B_modcol)
        tap("g1b", g1b, B_g1b)
        T.barrier(final=True)
        AR.top = PERSIST_TOP

        def ln_stats_g(src, B_src, st, mv, rstd, B_small):
            T.op("dve", lambda h: h.bn_stats(out=st[:, 0, :], in_=src[:, 0:512]), reads=[B_src], writes=[B_small])
            yield
            T.op("dve", lambda h: h.bn_stats(out=st[:, 1, :], in_=src[:, 512:1024]), reads=[B_src], writes=[B_small])
            yield
            T.op("dve", lambda h: h.bn_aggr(out=mv[:, 0:2], in_=st.rearrange("p a b -> p (a b)")), reads=[B_small], writes=[B_small])
            yield
            T.op("act", lambda h: h.activation(out=mv[:, 2:3], in_=mv[:, 1:2], func=AF.Sqrt, bias=epsln, scale=1.0),
                 reads=[B_small, B_cst], writes=[B_small])
            yield
            T.op("dve", lambda h: h.reciprocal(out=rstd, in_=mv[:, 2:3]), reads=[B_small], writes=[B_small])
            yield

        def ln_stats(*a):
            for _ in ln_stats_g(*a):
                pass

        def ln_mod_T_g(src, B_src, xn, B_xn, st, mv, B_small, colbase, dst_fn, B_dst):
            rstd = mv[:, 3:4]
            yield from ln_stats_g(src, B_src, st, mv, rstd, B_small)
            T.op("dve", lambda h: h.tensor_scalar(out=xn, in0=src, scalar1=mv[:, 0:1], scalar2=rstd, op0=ALU.subtract, op1=ALU.mult),
                 reads=[B_src, B_small], writes=[B_xn])
            yield
            ps, pb = next_ps()
            psb = ps.bitcast(BF16)

            def tr(h):
                for kc in range(8):
                    r = h.transpose(psb[:, kc * 128:(kc + 1) * 128], xn[:, kc * 128:(kc + 1) * 128], identB)
                return r
            T.op("pe", tr, reads=[B_xn, B_identB], writes=[pb])
            yield
            for kc in range(8):
                T.op("act", lambda h, kc=kc: h.activation(
                    out=dst_fn(kc), in_=psb[:, kc * 128:(kc + 1) * 128], func=AF.Identity,
                    scale=modcol[:, colbase + 8 + kc:colbase + 9 + kc], bias=modcol[:, colbase + kc:colbase + kc + 1]),
                    reads=[pb, B_modcol], writes=[B_dst])
                yield

        def ln_mod_T(*a):
            for _ in ln_mod_T_g(*a):
                pass

        def run_interleaved(gens):
            gens = [g for g in gens if g is not None]
            while gens:
                for g in list(gens):
                    try:
                        next(g)
                    except StopIteration:
                        gens.remove(g)

        hT = AR.alloc([8, TT], BF16)
        sz = AR.alloc([4, 2048], BF16)
        xbcT = AR.alloc([32, TT], BF16)
        xs_tok = AR.alloc([4, 2048], BF16)
        B_tok = AR.alloc([4, 1024], BF16)
        mergedT = B_tok.rearrange("p a b -> p (a b)").rearrange("p (a b) -> p a b", a=8)
        ubT = AR.alloc([8, TT], BF16)
        dtt = AR.alloc([4, 32])
        wblk = [AR.alloc([8, 512], BF16) for _ in range(2)]
        B_sz, B_xsT, B_BCT, B_ubT, B_dt = (
            Buf(n) for n in ("sz", "xsT", "BCT", "ubT", "dt"))
        B_hTs = [Buf(f"hT{i}") for i in range(4)]
        B_xstoks = [Buf(f"xstok{i}") for i in range(4)]
        B_Btoks = [Buf(f"Btok{i}") for i in range(4)]
        B_wblk = [Buf("wblk0"), Buf("wblk1")]
        wblk_sem = [new_dsem("wblk0"), new_dsem("wblk1")]
        wrr = [0]
        x_sem = [new_dsem("x0"), new_dsem("x1")]
        x1st_sem = [new_dsem("x1st0"), new_dsem("x1st1")]
        h2st_sem = [new_dsem("h2st0"), new_dsem("h2st1")]
        UNION_TOP = AR.top

        def load_w(src_ap, shape3):
            sl = wrr[0] % 2
            wrr[0] += 1
            flat = wblk[sl].rearrange("p a b -> p (a b)")
            n = shape3[0] * shape3[1]
            v = flat[:, 0:n].rearrange("p (a b) -> p a b", a=shape3[0])
            T.dma("pool", [lambda h, v=v, src_ap=src_ap: h.dma_start(out=v, in_=src_ap)], wblk_sem[sl], writes=[B_wblk[sl]])
            return v, B_wblk[sl]

        def win_cols(c0, n):
            return w_in_d[:, c0:c0 + n].rearrange("(kc p) n -> p kc n", p=128)

        def proj_fm(wv, wb, j, ps, pb):
            def mm(h):
                for kc in range(8):
                    r = h.matmul(ps, wv[:, kc, j * 128:(j + 1) * 128], hT[:, kc, :], start=(kc == 0), stop=(kc == 7))
                return r
            T.op("pe", mm, reads=[wb] + B_hTs, writes=[pb])

        for t in range(n_tiles):
            def stage_P(t=t):
                AR.top = UNION_TOP
                xt = [AR.alloc([D]) for _ in range(2)]
                xn = AR.alloc([D], BF16)
                st6 = AR.alloc([2, 6])
                mv = AR.alloc([8])
                pre = [AR.alloc([TT + 3]) for _ in range(2)]
                preb = [AR.alloc([TT + 4], BF16) for _ in range(2)]
                diag = [AR.alloc([4, 128], BF16) for _ in range(2)]
                sccbuf = AR.alloc([4, TT])
                dtmp = AR.alloc([2, 32])
                B_xt = [Buf("xt0"), Buf("xt1")]
                B_xn, B_small = Buf("xn"), Buf("lnsmall")
                B_pre = [Buf("pre0"), Buf("pre1")]
                B_preb = [Buf("preb0"), Buf("preb1")]
                B_diag = [Buf("diag0"), Buf("diag1")]
                B_scc = [Buf(f"scc{j}") for j in range(4)]
                B_dtmp = Buf("dtmp")
                for ci in range(4):
                    tok0 = t * TT + ci * 128
                    sl = ci % 2
                    T.dma("sp", [lambda h, sl=sl, tok0=tok0: h.dma_start(out=xt[sl], in_=x_d[tok0:tok0 + 128, :])], x_sem[sl], writes=[B_xt[sl]])
                    ln_mod_T(xt[sl], B_xt[sl], xn, B_xn, st6, mv, B_small, 0,
                             lambda kc, ci=ci: hT[:, kc, ci * 128:(ci + 1) * 128], B_hTs[ci])
                    if t == 0 and ci == 0:
                        tap("xt0", xt[sl], B_xt[sl])
                        tap("xn0", xn, B_xn)
                        tap("mv0", mv, B_small)
                        if cfg.get("stop") == "ln0":
                            tap("hT", hT, B_hTs[3])
                            return 'stop'
                if t == 0:
                    tap("hT", hT, B_hTs[3])
                for blk in range(4):
                    wv, wb = load_w(win_cols(C_Z + blk * 512, 512), (8, 512))
                    for ci in range(4):
                        ps, pb = next_ps()

                        def mm(h, ps=ps, wv=wv, ci=ci):
                            for kc in range(8):
                                r = h.matmul(ps, hT[:, kc, ci * 128:(ci + 1) * 128], wv[:, kc, :], start=(kc == 0), stop=(kc == 7))
                            return r
                        T.op("pe", mm, reads=[wb, B_hTs[ci]], writes=[pb])
                        T.op("act", lambda h, ps=ps, ci=ci, blk=blk: h.activation(out=sz[:, ci, blk * 512:(blk + 1) * 512], in_=ps, func=AF.Silu),
                             reads=[pb], writes=[B_sz])
                wcur = [None]

                def xbc_front(idx):
                    blk, j = idx // 4, idx % 4
                    if j == 0:
                        wcur[0] = load_w(win_cols(C_XBC + blk * 512, 512), (8, 512))
                    wv, wb = wcur[0]
                    ch = idx
                    sl = idx % 2
                    ps, pb = next_ps()
                    proj_fm(wv, wb, j, ps, pb)
                    T.op("act", lambda h: h.copy(out=preb[sl][:, 3:TT + 3], in_=ps), reads=[pb], writes=[B_preb[sl]])
                    T.op("dve", lambda h: h.tensor_copy(out=preb[sl][:, 0:3], in_=halo[:, ch, :]), reads=[B_halo[ch]], writes=[B_preb[sl]])
                    T.op("dve", lambda h: h.tensor_copy(out=halo[:, ch, :], in_=preb[sl][:, TT:TT + 3]), reads=[B_preb[sl]], writes=[B_halo[ch]])
                    cw0 = CV_CW + ch * 4
                    for k in range(4):
                        T.op("dve", lambda h, k=k: h.tensor_scalar(
                            out=diag[sl][:, k, :], in0=identB, scalar1=colv[:, cw0 + k:cw0 + k + 1], scalar2=None, op0=ALU.mult),
                            reads=[B_identB, B_colv], writes=[B_diag[sl]])

                def xbc_back(idx):
                    ch = idx
                    sl = idx % 2
                    ps2, pb2 = next_ps()

                    def mmc(h):
                        for k in range(4):
                            r = h.matmul(ps2, diag[sl][:, k, :], preb[sl][:, k:k + TT], start=(k == 0), stop=(k == 3))
                        return r
                    T.op("pe", mmc, reads=[B_diag[sl], B_preb[sl]], writes=[pb2])
                    T.op("act", lambda h: h.activation(
                        out=xbcT[:, ch, :], in_=ps2, func=AF.Silu, bias=colv[:, CV_CB + ch:CV_CB + ch + 1], scale=1.0),
                        reads=[pb2, B_colv], writes=[B_xsT if ch < 16 else B_BCT])

                xbc_front(0)
                for idx in range(32):
                    if idx + 1 < 32:
                        xbc_front(idx + 1)
                    xbc_back(idx)
                wv, wb = load_w(win_cols(C_DT, 32), (8, 32))
                for ci in range(4):
                    ps, pb = next_ps()

                    def mm(h, ps=ps, wv=wv, ci=ci):
                        for kc in range(8):
                            r = h.matmul(ps[:, 0:32], hT[:, kc, ci * 128:(ci + 1) * 128], wv[:, kc, :], start=(kc == 0), stop=(kc == 7))
                        return r
                    T.op("pe", mm, reads=[wb, B_hTs[ci]], writes=[pb])
                    T.op("dve", lambda h, ps=ps: h.tensor_tensor(out=dtmp[:, 0, :], in0=ps[:, 0:32], in1=rowv[:, R_DTB:R_DTB + 32], op=ALU.add),
                         reads=[pb, B_rowv], writes=[B_dtmp])
                    T.op("act", lambda h: h.activation(out=dtmp[:, 1, :], in_=dtmp[:, 0, :], func=AF.Exp), reads=[B_dtmp], writes=[B_dtmp])
                    T.op("act", lambda h, ci=ci: h.activation(out=dtt[:, ci, :], in_=dtmp[:, 1, :], func=AF.Ln, bias=onecol, scale=1.0),
                         reads=[B_dtmp, B_cst], writes=[B_dt])
                for k2 in range(2):
                    wv, wb = load_w(win_cols(C_SCC + k2 * 512, 512), (8, 512))
                    for j in range(4):
                        ps, pb = next_ps()
                        proj_fm(wv, wb, j, ps, pb)
                        T.op("act", lambda h, ps=ps, j=j: h.copy(out=sccbuf[:, j, :], in_=ps), reads=[pb], writes=[B_scc[j]])
                    wv, wb = load_w(win_cols(C_SCH + k2 * 512, 512), (8, 512))
                    for j in range(4):
                        ch8 = k2 * 4 + j
                        ps, pb = next_ps()
                        proj_fm(wv, wb, j, ps, pb)
                        sl = j % 2
                        T.op("dve", lambda h, ps=ps, j=j, sl=sl: h.tensor_tensor(out=pre[sl][:, 2:TT + 2], in0=ps, in1=sccbuf[:, j, :], op=ALU.mult),
                             reads=[pb, B_scc[j]], writes=[B_pre[sl]])
                        T.op("dve", lambda h, sl=sl, ch8=ch8: h.tensor_copy(out=pre[sl][:, 0:2], in_=halo2[:, ch8, :]), reads=[B_halo2[ch8]], writes=[B_pre[sl]])
                        T.op("dve", lambda h, sl=sl, ch8=ch8: h.tensor_copy(out=halo2[:, ch8, :], in_=pre[sl][:, TT:TT + 2]), reads=[B_pre[sl]], writes=[B_halo2[ch8]])
                        c0 = CV_SCW + ch8 * 3
                        T.op("dve", lambda h, sl=sl, j=j, c0=c0: h.tensor_scalar(
                            out=sccbuf[:, j, :], in0=pre[sl][:, 0:TT], scalar1=colv[:, c0:c0 + 1], scalar2=None, op0=ALU.mult),
                            reads=[B_pre[sl], B_colv], writes=[B_scc[j]])
                        for k in range(1, 3):
                            T.op("dve", lambda h, sl=sl, j=j, k=k, c0=c0: h.scalar_tensor_tensor(
                                out=sccbuf[:, j, :], in0=pre[sl][:, k:k + TT], scalar=colv[:, c0 + k:c0 + k + 1], in1=sccbuf[:, j, :],
                                op0=ALU.mult, op1=ALU.add), reads=[B_pre[sl], B_colv, B_scc[j]], writes=[B_scc[j]])
                    wv, wb = load_w(win_cols(C_SCB + k2 * 512, 512), (8, 512))
                    for j in range(4):
                        ch8 = k2 * 4 + j
                        ps, pb = next_ps()
                        proj_fm(wv, wb, j, ps, pb)
                        T.op("dve", lambda h, ps=ps, j=j, ch8=ch8: h.tensor_tensor(out=ubT[:, ch8, :], in0=ps, in1=sccbuf[:, j, :], op=ALU.mult),
                             reads=[pb, B_scc[j]], writes=[B_ubT])
                if t == 0:
                    tap("hT_end", hT, B_hTs[3])
                    tap("sz", sz, B_sz)
                    tap("xsT", xbcT[:, 0:16, :], B_xsT)
                    tap("BCT", xbcT[:, 16:32, :], B_BCT)
                    tap("dt", dtt, B_dt)
                    tap("ubT", ubT, B_ubT)
                T.barrier()
                if cfg.get("stop") == "P":
                    return 'stop'
                return None

            if stage_P() == 'stop':
                break
            def stage_S(t=t):
                AR.top = UNION_TOP
                dA = AR.alloc([32])
                sm = AR.alloc([6, 32])
                triDA = [AR.alloc([4, 128]) for _ in range(3)]
                cbm = AR.alloc([8, 128])
                LT = [AR.alloc([4, 128]) for _ in range(2)]
                scTt = [AR.alloc([4, 128], BF16) for _ in range(2)]
                gtmp = AR.alloc([2048])
                Xb = gtmp[:, 0:1024].bitcast(BF16).rearrange("p (a b) -> p a b", a=32)
                Xdec = gtmp[:, 1024:2048].bitcast(BF16).rearrange("p (a b) -> p a b", a=32)
                ytok = AR.alloc([32, 64])
                ytmp = [AR.alloc([4, 64]) for _ in range(2)]
                gn_tok = AR.alloc([2048], BF16)
                rms = AR.alloc([24])
                B_dA, B_sm, B_cbm, B_X, B_ytok, B_gntok, B_rms = (
                    Buf(n) for n in ("dA", "sm", "cbm", "XG", "ytok", "gntok", "rms"))
                B_Xdec = B_X
                B_gtmp = B_X
                B_triDA = [Buf("triDA0"), Buf("triDA1"), Buf("triDA2")]
                B_LT = [Buf("LT0"), Buf("LT1")]
                B_scTt = [Buf("scT0"), Buf("scT1")]
                B_ytmp = [Buf("ytmp0"), Buf("ytmp1")]
                def trans(ci):
                    for grp8 in range(3):
                        ps, pb = next_ps()
                        psb = ps.bitcast(BF16)

                        def tr(h, psb=psb, grp8=grp8):
                            for q in range(8):
                                ch = grp8 * 8 + q
                                r = h.transpose(psb[:, q * 128:(q + 1) * 128], xbcT[:, ch, ci * 128:(ci + 1) * 128], identB)
                            return r
                        T.op("pe", tr, reads=[B_xsT if grp8 < 2 else B_BCT, B_identB], writes=[pb])
                        if grp8 < 2:
                            T.op("act", lambda h, psb=psb, grp8=grp8: h.copy(out=xs_tok[:, ci, grp8 * 1024:(grp8 + 1) * 1024], in_=psb),
                                 reads=[pb], writes=[B_xstoks[ci]])
                        else:
                            T.op("act", lambda h, psb=psb: h.copy(out=B_tok[:, ci, :], in_=psb), reads=[pb], writes=[B_Btoks[ci]])
                if cfg.get("stop") == "S_tr":
                    for ci in range(4):
                        trans(ci)
                else:
                    trans(0)
                if cfg.get("stop") == "S_tr":
                    tap("xstok", xs_tok, B_xstoks[3])
                    T.barrier()
                    return 'stop'
                gcount = 0
                for ci in range(4):
                    cs = slice(ci * 128, (ci + 1) * 128)
                    xs3 = xs_tok[:, ci, :].rearrange("p (a b) -> p a b", a=32)
                    T.op("dve", lambda h, ci=ci: h.tensor_tensor(out=dA, in0=dtt[:, ci, :], in1=negA, op=ALU.mult), reads=[B_dt, B_negA], writes=[B_dA])
                    ps, pb = next_ps()

                    def mmA(h, ps=ps):
                        h.matmul(ps[:, 0:32], triU, dA, start=True, stop=True)
                        return h.matmul(ps[:, 32:64], onesF, dA, start=True, stop=True)
                    T.op("pe", mmA, reads=[B_cst, B_dA], writes=[pb])
                    T.op("act", lambda h, ps=ps: h.copy(out=sm[:, 0, :], in_=ps[:, 0:32]), reads=[pb], writes=[B_sm])
                    T.op("act", lambda h, ps=ps: h.activation(out=sm[:, 1, :], in_=ps[:, 0:32], func=AF.Exp), reads=[pb], writes=[B_sm])
                    T.op("act", lambda h, ps=ps: h.activation(out=sm[:, 3, :], in_=ps[:, 32:64], func=AF.Exp), reads=[pb], writes=[B_sm])
                    T.op("dve", lambda h, ps=ps: h.tensor_tensor(out=sm[:, 2, :], in0=ps[:, 32:64], in1=sm[:, 0, :], op=ALU.subtract), reads=[pb, B_sm], writes=[B_sm])
                    T.op("act", lambda h: h.activation(out=sm[:, 2, :], in_=sm[:, 2, :], func=AF.Exp), reads=[B_sm], writes=[B_sm])
                    T.op("dve", lambda h, ci=ci: h.tensor_tensor(out=sm[:, 4, :], in0=sm[:, 2, :], in1=dtt[:, ci, :], op=ALU.mult), reads=[B_sm, B_dt], writes=[B_sm])
                    T.op("dve", lambda h, xs3=xs3, ci=ci: h.tensor_tensor(
                        out=Xb, in0=xs3, in1=dtt[:, ci, :].unsqueeze(2).to_broadcast([128, 32, 64]), op=ALU.mult), reads=[B_xstoks[ci], B_dt], writes=[B_X])
                    T.op("dve", lambda h, xs3=xs3: h.tensor_tensor(
                        out=Xdec, in0=xs3, in1=sm[:, 4, :].unsqueeze(2).to_broadcast([128, 32, 64]), op=ALU.mult), reads=[B_xstoks[ci], B_sm], writes=[B_Xdec])
                    if cfg.get("stop") == "S_pre":
                        tap("sm", sm, B_sm)
                        tap("Xb", Xb, B_X)
                        T.barrier()
                        return 'stop'
                    for half in range(2):
                        ps, pb = next_ps()

                        def mmcb(h, ps=ps, half=half, cs=cs):
                            for q in range(4):
                                g = half * 4 + q
                                r = h.matmul(ps[:, q * 128:(q + 1) * 128], xbcT[:, 16 + g, cs], xbcT[:, 24 + g, cs], start=True, stop=True)
                            return r
                        T.op("pe", mmcb, reads=[B_BCT], writes=[pb])
                        T.op("dve", lambda h, ps=ps, half=half: h.tensor_tensor(
                            out=cbm[:, half * 4:(half + 1) * 4, :], in0=ps.rearrange("p (a b) -> p a b", a=4),
                            in1=triU.unsqueeze(1).to_broadcast([128, 4, 128]), op=ALU.mult), reads=[pb, B_cst], writes=[B_cbm])
                    if cfg.get("stop") == "S_cb":
                        tap("cbm", cbm, B_cbm)
                        T.barrier()
                        return 'stop'
                    def front0(g):
                        s3 = g % 3
                        hs = slice(4 * g, 4 * g + 4)
                        T.op("pool", lambda h: h.tensor_tensor(
                            out=triDA[s3], in0=triU.unsqueeze(1).to_broadcast([128, 4, 128]),
                            in1=dA[:, hs].unsqueeze(2).to_broadcast([128, 4, 128]), op=ALU.mult), reads=[B_cst, B_dA], writes=[B_triDA[s3]])

                    def front1(g):
                        sl = g % 2
                        s3 = g % 3
                        ps_s, pb_s = next_ps()
                        T.op("pe", lambda h: h.matmul(ps_s, striL, triDA[s3].rearrange("p a b -> p (a b)"), start=True, stop=True),
                             reads=[B_cst, B_triDA[s3]], writes=[pb_s])
                        T.op("act", lambda h: h.activation(out=LT[sl].rearrange("p a b -> p (a b)"), in_=ps_s, func=AF.Exp),
                             reads=[pb_s], writes=[B_LT[sl]])

                    def front2(g):
                        sl = g % 2
                        T.op("dve", lambda h, sl=sl, g=g: h.tensor_tensor(
                            out=scTt[sl], in0=LT[sl], in1=cbm[:, g, :].unsqueeze(1).to_broadcast([128, 4, 128]), op=ALU.mult),
                            reads=[B_LT[sl], B_cbm], writes=[B_scTt[sl]])

                    def back(g, cs=cs, ci=ci):
                        sl = g % 2
                        hs = slice(4 * g, 4 * g + 4)
                        ps_y, pb_y = next_ps()

                        def mmy(h, ps_y=ps_y, g=g, sl=sl, hs=hs, cs=cs):
                            h.matmul(ps_y[:, 0:256], xbcT[:, 24 + g, cs], stbf[:, hs, :].rearrange("p a b -> p (a b)"), start=True, stop=True)
                            for hh in range(4):
                                r = h.matmul(ps_y[:, 256 + hh * 64:256 + (hh + 1) * 64], scTt[sl][:, hh, :], Xb[:, 4 * g + hh, :], start=True, stop=True)
                            return r
                        T.op("pe", mmy, reads=[B_BCT, B_stbf[g], B_scTt[sl], B_X], writes=[pb_y])
                        ps_n, pb_n = next_ps()
                        T.op("pe", lambda h, ps_n=ps_n, g=g, ci=ci, hs=hs: h.matmul(
                            ps_n[:, 0:256], B_tok[:, ci, g * 128:(g + 1) * 128], Xdec[:, hs, :].rearrange("p a b -> p (a b)"), start=True, stop=True),
                            reads=[B_Btoks[ci], B_Xdec], writes=[pb_n])
                        return sl, hs, ps_y, pb_y, ps_n, pb_n

                    def back2(g, sl, hs, ps_y, pb_y, ps_n, pb_n):
                        T.op("dve", lambda h: h.tensor_tensor(
                            out=ytmp[sl], in0=ps_y[:, 0:256].rearrange("p (a b) -> p a b", a=4),
                            in1=sm[:, 1, hs].unsqueeze(2).to_broadcast([128, 4, 64]), op=ALU.mult), reads=[pb_y, B_sm], writes=[B_ytmp[sl]])
                        T.op("dve", lambda h: h.tensor_tensor(
                            out=ytok[:, hs, :], in0=ps_y[:, 256:512].rearrange("p (a b) -> p a b", a=4), in1=ytmp[sl], op=ALU.add),
                            reads=[pb_y, B_ytmp[sl]], writes=[B_ytok])
                        T.op("dve", lambda h: h.tensor_tensor(
                            out=state[:, hs, :], in0=state[:, hs, :], in1=sm[:, 3, hs].unsqueeze(2).to_broadcast([128, 4, 64]), op=ALU.mult),
                            reads=[B_state[g], B_sm], writes=[B_state[g]])
                        T.op("dve", lambda h: h.tensor_tensor(
                            out=state[:, hs, :], in0=state[:, hs, :], in1=ps_n[:, 0:256].rearrange("p (a b) -> p a b", a=4), op=ALU.add),
                            reads=[B_state[g], pb_n], writes=[B_state[g]])
                        T.op("act", lambda h: h.copy(out=stbf[:, hs, :], in_=state[:, hs, :]), reads=[B_state[g]], writes=[B_stbf[g]])

                    front0(0)
                    front0(1)
                    front1(0)
                    front2(0)
                    for g in range(8):
                        if g + 2 < 8:
                            front0(g + 2)
                        if g + 1 < 8:
                            front1(g + 1)
                        bk = back(g)
                        if g + 1 < 8:
                            front2(g + 1)
                        back2(g, *bk)
                    if ci + 1 < 4:
                        trans(ci + 1)
                    if cfg.get("stop") == "S_grp":
                        tap("ytok", ytok, B_ytok)
                        T.barrier()
                        return 'stop'
                    g3 = gtmp.rearrange("p (a b) -> p a b", a=32)
                    T.op("dve", lambda h, xs3=xs3, g3=g3: h.tensor_tensor(
                        out=g3, in0=xs3, in1=rowv[:, R_D:R_D + 32].unsqueeze(2).to_broadcast([128, 32, 64]), op=ALU.mult),
                        reads=[B_xstoks[ci], B_rowv], writes=[B_gtmp])
                    T.op("dve", lambda h, g3=g3: h.tensor_tensor(out=ytok, in0=ytok, in1=g3, op=ALU.add), reads=[B_ytok, B_gtmp], writes=[B_ytok])
                    if t == 0 and ci == 1:
                        tap("y1", ytok, B_ytok)
                    T.op("dve", lambda h, ci=ci: h.tensor_tensor(out=gtmp, in0=ytok.rearrange("p a b -> p (a b)"), in1=sz[:, ci, :], op=ALU.mult),
                         reads=[B_ytok, B_sz], writes=[B_gtmp])
                    for g in range(8):
                        T.op("act", lambda h, g=g: h.activation(
                            out=ytok.rearrange("p a b -> p (a b)")[:, g * 256:(g + 1) * 256], in_=gtmp[:, g * 256:(g + 1) * 256],
                            func=AF.Square, accum_out=rms[:, g:g + 1]), reads=[B_gtmp], writes=[B_ytok, B_rms])
                    T.op("act", lambda h: h.activation(out=rms[:, 8:16], in_=rms[:, 0:8], func=AF.Sqrt, bias=epsrms, scale=1.0 / 256.0),
                         reads=[B_rms, B_cst], writes=[B_rms])
                    T.op("dve", lambda h: h.reciprocal(out=rms[:, 16:24], in_=rms[:, 8:16]), reads=[B_rms], writes=[B_rms])
                    for g in range(8):
                        T.op("dve", lambda h, g=g: h.scalar_tensor_tensor(
                            out=gn_tok[:, g * 256:(g + 1) * 256], in0=gtmp[:, g * 256:(g + 1) * 256], scalar=rms[:, 16 + g:17 + g],
                            in1=rowv[:, R_NW + g * 256:R_NW + (g + 1) * 256], op0=ALU.mult, op1=ALU.mult),
                            reads=[B_gtmp, B_rms, B_rowv], writes=[B_gntok])
                    if t == 0 and ci == 1:
                        tap("gn1", gn_tok, B_gntok)
                    if cfg.get("stop") == "S_gn":
                        tap("gntok", gn_tok, B_gntok)
                        T.barrier()
                        return 'stop'
                    for grp8 in range(2):
                        ps, pb = next_ps()
                        psb = ps.bitcast(BF16)

                        def tr2(h, psb=psb, grp8=grp8):
                            for q in range(8):
                                c0 = (grp8 * 8 + q) * 128
                                r = h.transpose(psb[:, q * 128:(q + 1) * 128], gn_tok[:, c0:c0 + 128], identB)
                            return r
                        T.op("pe", tr2, reads=[B_gntok, B_identB], writes=[pb])
                        T.op("act", lambda h, psb=psb, grp8=grp8, cs=cs: h.copy(
                            out=xbcT[:, grp8 * 8:(grp8 + 1) * 8, cs], in_=psb.rearrange("p (a b) -> p a b", a=8)),
                            reads=[pb], writes=[B_xsT])
                T.barrier()

            if stage_S() == 'stop':
                break
            def stage_O(t=t):
                AR.top = UNION_TOP
                gnT = xbcT
                sg = [AR.alloc([TT]) for _ in range(2)]
                m1 = [AR.alloc([TT]) for _ in range(4)]
                u = [AR.alloc([D]) for _ in range(2)]
                xt = [AR.alloc([D]) for _ in range(2)]
                xn = AR.alloc([D], BF16)
                st6 = AR.alloc([2, 6])
                mv = AR.alloc([8])
                h2t = [AR.alloc([8, 128], BF16) for _ in range(2)]
                st6a = [AR.alloc([2, 6]) for _ in range(2)]
                mva = [AR.alloc([8]) for _ in range(2)]
                B_smalla = [Buf("lnsmallA0"), Buf("lnsmallA1")]
                rt = AR.alloc([8, 64])
                m8 = AR.alloc([8, 8])
                rsm = AR.alloc([40])
                B_sg = [Buf("sg0"), Buf("sg1")]
                B_m1 = [Buf(f"m1{j}") for j in range(4)]
                B_u = [Buf("u0"), Buf("u1")]
                B_xt = [Buf("xt0"), Buf("xt1")]
                B_xn, B_small = Buf("xn"), Buf("lnsmall")
                B_h2t = [Buf("h2t0"), Buf("h2t1")]
                B_rt, B_m8, B_rsm = Buf("rt"), Buf("m8"), Buf("rsm")
                for half in range(2):
                    wga, bga = load_w(win_cols(C_GA + half * 512, 512), (8, 512))
                    for j in range(4):
                        ps, pb = next_ps()
                        proj_fm(wga, bga, j, ps, pb)
                        T.op("act", lambda h, ps=ps, j=j: h.activation(out=m1[j], in_=ps, func=AF.Sigmoid), reads=[pb], writes=[B_m1[j]])
                    wso = []
                    for pair in range(2):
                        c0 = half * 512 + pair * 256
                        wso.append(load_w(w_sso_d[:, c0:c0 + 256].rearrange("(kc p) n -> p kc n", p=128), (16, 256)))
                    for j in range(4):
                        wv, wb = wso[j // 2]
                        ps2, pb2 = next_ps()

                        def mma(h, ps2=ps2, wv=wv, j=j):
                            for kc in range(16):
                                r = h.matmul(ps2, wv[:, kc, (j % 2) * 128:(j % 2 + 1) * 128], gnT[:, kc, :], start=(kc == 0), stop=(kc == 15))
                            return r
                        T.op("pe", mma, reads=[wb, B_xsT], writes=[pb2])
                        T.op("dve", lambda h, ps2=ps2, j=j: h.tensor_tensor(out=m1[j], in0=ps2, in1=m1[j], op=ALU.mult),
                             reads=[pb2, B_m1[j]], writes=[B_m1[j]])
                    wgb, bgb = load_w(win_cols(C_GB + half * 512, 512), (8, 512))
                    wsc, bsc = load_w(w_sco_d[:, half * 512:(half + 1) * 512].rearrange("(kc p) n -> p kc n", p=128), (8, 512))
                    for j in range(4):
                        dj = half * 4 + j
                        sl = j % 2
                        ps3, pb3 = next_ps()
                        proj_fm(wgb, bgb, j, ps3, pb3)
                        T.op("act", lambda h, ps3=ps3, sl=sl: h.activation(out=sg[sl], in_=ps3, func=AF.Sigmoid), reads=[pb3], writes=[B_sg[sl]])
                        ps4, pb4 = next_ps()

                        def mmb(h, ps4=ps4, wsc=wsc, j=j):
                            for kc in range(8):
                                r = h.matmul(ps4, wsc[:, kc, j * 128:(j + 1) * 128], ubT[:, kc, :], start=(kc == 0), stop=(kc == 7))
                            return r
                        T.op("pe", mmb, reads=[bsc, B_ubT], writes=[pb4])
                        T.op("dve", lambda h, ps4=ps4, sl=sl: h.tensor_tensor(out=sg[sl], in0=ps4, in1=sg[sl], op=ALU.mult),
                             reads=[pb4, B_sg[sl]], writes=[B_sg[sl]])
                        T.op("dve", lambda h, sl=sl, dj=dj, j=j: h.tensor_tensor(out=mergedT[:, dj, :], in0=m1[j], in1=sg[sl], op=ALU.add),
                             reads=[B_m1[j], B_sg[sl]], writes=B_Btoks)
                if t == 0:
                    tap("mergedT", mergedT, B_Btoks[3])
                wo0, bo0 = load_w(w_o_d[:, 0:512].rearrange("(kc p) n -> p kc n", p=128), (8, 512))
                wo1, bo1 = load_w(w_o_d[:, 512:1024].rearrange("(kc p) n -> p kc n", p=128), (8, 512))

                def partA(ci):
                    chunk = t * 4 + ci
                    tok0 = chunk * 128
                    sl = ci % 2
                    T.dma("sp", [lambda h: h.dma_start(out=xt[sl], in_=x_d[tok0:tok0 + 128, :])], x_sem[sl], writes=[B_xt[sl]])
                    yield
                    for hf, (wo, bo) in enumerate(((wo0, bo0), (wo1, bo1))):
                        ps, pb = next_ps()

                        def mmo(h, ps=ps, wo=wo):
                            for kc in range(8):
                                r = h.matmul(ps, mergedT[:, kc, ci * 128:(ci + 1) * 128], wo[:, kc, :], start=(kc == 0), stop=(kc == 7))
                            return r
                        T.op("pe", mmo, reads=[bo] + B_Btoks, writes=[pb])
                        yield
                        T.op("dve", lambda h, ps=ps, hf=hf: h.tensor_tensor(
                            out=u[sl][:, hf * 512:(hf + 1) * 512], in0=ps, in1=g1b[:, hf * 512:(hf + 1) * 512], op=ALU.mult),
                            reads=[pb, B_g1b], writes=[B_u[sl]])
                        yield
                    if t == 0 and ci == 1:
                        tap("gmix1", u[sl], B_u[sl])
                    T.op("dve", lambda h: h.scalar_tensor_tensor(out=u[sl], in0=xt[sl], scalar=float(ALPHA), in1=u[sl], op0=ALU.mult, op1=ALU.add),
                         reads=[B_xt[sl], B_u[sl]], writes=[B_u[sl]])
                    yield
                    yield from ln_stats_g(u[sl], B_u[sl], st6a[sl], mva[sl], mva[sl][:, 3:4], B_smalla[sl])
                    T.op("dve", lambda h: h.tensor_scalar(out=u[sl], in0=u[sl], scalar1=mva[sl][:, 0:1], scalar2=mva[sl][:, 3:4], op0=ALU.subtract, op1=ALU.mult),
                         reads=[B_u[sl], B_smalla[sl]], writes=[B_u[sl]])
                    yield
                    T.op("dve", lambda h: h.tensor_tensor(out=u[sl], in0=u[sl], in1=rowv[:, R_L1G:R_L1G + D], op=ALU.mult), reads=[B_u[sl], B_rowv], writes=[B_u[sl]])
                    yield
                    T.op("dve", lambda h: h.tensor_tensor(out=u[sl], in0=u[sl], in1=rowv[:, R_L1B:R_L1B + D], op=ALU.add), reads=[B_u[sl], B_rowv], writes=[B_u[sl]])
                    yield
                    T.dma("sp", [lambda h: h.dma_start(out=x1_d[tok0:tok0 + 128, :], in_=u[sl])], x1st_sem[sl], reads=[B_u[sl]])
                    yield
                    if t == 0 and ci == 1:
                        tap("x1_1", u[sl], B_u[sl])

                def partB(ci):
                    chunk = t * 4 + ci
                    tok0 = chunk * 128
                    sl = ci % 2
                    yield from ln_mod_T_g(u[sl], B_u[sl], xn, B_xn, st6, mv, B_small, 16, lambda kc: h2t[sl][:, kc, :], B_h2t[sl])
                    T.dma("sp", [lambda h: h.dma_start(out=h2T_d[:, :, tok0:tok0 + 128], in_=h2t[sl])], h2st_sem[sl], reads=[B_h2t[sl]])
                    yield
                    if t == 0 and ci == 1:
                        tap("h2t1", h2t[sl], B_h2t[sl])
                    ps, pb = next_ps()

                    def mmr(h):
                        for kc in range(8):
                            r = h.matmul(ps[:, 0:64], h2t[sl][:, kc, :], rw[:, kc, :], start=(kc == 0), stop=(kc == 7))
                        return r
                    T.op("pe", mmr, reads=[B_h2t[sl], B_rw], writes=[pb])
                    yield
                    sc_, bi_, mk_, se_ = rt[:, 0, :], rt[:, 1, :], rt[:, 2, :], rt[:, 3, :]
                    T.op("act", lambda h: h.activation(out=sc_, in_=ps[:, 0:64], func=AF.Sigmoid), reads=[pb], writes=[B_rt])
                    yield
                    T.op("dve", lambda h: h.tensor_tensor(out=bi_, in0=sc_, in1=rowv[:, R_RB:R_RB + 64], op=ALU.add), reads=[B_rt, B_rowv], writes=[B_rt])
                    yield
                    for g in range(8):
                        T.op("dve", lambda h, g=g: h.max(out=m8[:, g, :], in_=bi_[:, g * 8:(g + 1) * 8]), reads=[B_rt], writes=[B_m8])
                        yield
                    T.op("dve", lambda h: h.tensor_tensor(out=rsm[:, 0:8], in0=m8[:, :, 0], in1=m8[:, :, 1], op=ALU.add), reads=[B_m8], writes=[B_rsm])
                    yield
                    T.op("dve", lambda h: h.max(out=rsm[:, 8:16], in_=rsm[:, 0:8]), reads=[B_rsm], writes=[B_rsm])
                    yield
                    T.op("dve", lambda h: h.tensor_scalar(out=rsm[:, 16:24], in0=rsm[:, 0:8], scalar1=rsm[:, 11:12], scalar2=None, op0=ALU.is_ge),
                         reads=[B_rsm], writes=[B_rsm])
                    yield
                    T.op("dve", lambda h: h.tensor_scalar(out=rsm[:, 24:32], in0=rsm[:, 16:24], scalar1=-1.0, scalar2=1.0e9, op0=ALU.add, op1=ALU.mult),
                         reads=[B_rsm], writes=[B_rsm])
                    yield
                    mk3 = mk_.rearrange("p (a b) -> p a b", a=8)
                    T.op("dve", lambda h: h.tensor_tensor(
                        out=mk3, in0=bi_.rearrange("p (a b) -> p a b", a=8), in1=rsm[:, 16:24].unsqueeze(2).to_broadcast([128, 8, 8]), op=ALU.mult),
                        reads=[B_rt, B_rsm], writes=[B_rt])
                    yield
                    T.op("dve", lambda h: h.tensor_tensor(
                        out=mk3, in0=mk3, in1=rsm[:, 24:32].unsqueeze(2).to_broadcast([128, 8, 8]), op=ALU.add), reads=[B_rt, B_rsm], writes=[B_rt])
                    yield
                    T.op("dve", lambda h: h.max(out=rsm[:, 8:16], in_=mk_), reads=[B_rt], writes=[B_rsm])
                    yield
                    T.op("dve", lambda h: h.tensor_scalar(out=se_, in0=mk_, scalar1=rsm[:, 15:16], scalar2=None, op0=ALU.is_ge), reads=[B_rt, B_rsm], writes=[B_rt])
                    yield
                    T.op("dve", lambda h: h.tensor_tensor(out=se_, in0=se_, in1=sc_, op=ALU.mult), reads=[B_rt], writes=[B_rt])
                    yield
                    T.op("dve", lambda h: h.tensor_reduce(out=rsm[:, 32:33], in_=se_, axis=AX.X, op=ALU.add), reads=[B_rt], writes=[B_rsm])
                    yield
                    T.op("dve", lambda h: h.reciprocal(out=rsm[:, 33:34], in_=rsm[:, 32:33]), reads=[B_rsm], writes=[B_rsm])
                    yield
                    T.op("dve", lambda h: h.tensor_scalar(
                        out=G_all[:, chunk, 0:64], in0=se_, scalar1=rsm[:, 33:34], scalar2=2.5, op0=ALU.mult, op1=ALU.mult),
                        reads=[B_rt, B_rsm], writes=[B_G])
                    yield

                for step in range(5):
                    run_interleaved([partA(step) if step < 4 else None, partB(step - 1) if step >= 1 else None])
                T.barrier()
            if stage_O() == 'stop':
                break
        tap("G", G_all, B_G)

        def phase_B():
            T.barrier(final=True)
            AR.top = PERSIST_TOP
            NB = 2 if n_tiles == NTILE else 1
            BT = S // 2 if n_tiles == NTILE else n_tiles * TT
            nsub = BT // 128
            n512 = BT // 512
            h2blk = AR.alloc([8, BT], BF16)
            acc = AR.alloc([nsub, D])
            wgu = [AR.alloc([8, 512], BF16) for _ in range(2)]
            wdn = [AR.alloc([2, D], BF16) for _ in range(2)]
            sgt = [AR.alloc([512]) for _ in range(2)]
            actT = [AR.alloc([2, 512], BF16) for _ in range(2)]
            xt = [AR.alloc([D]) for _ in range(2)]
            st6 = AR.alloc([2, 6])
            mv = AR.alloc([8])
            B_h2blk, B_small = Buf("h2blk"), Buf("lnsmallB")
            B_acc = [Buf(f"acc{i}") for i in range(nsub)]
            B_wgu = [Buf("wgu0"), Buf("wgu1")]
            B_wdn = [Buf("wdn0"), Buf("wdn1")]
            B_sgt = [Buf("sgt0"), Buf("sgt1")]
            B_actT = [Buf("actT0"), Buf("actT1")]
            B_xt = [Buf("xtB0"), Buf("xtB1")]
            wgu_sem = [new_dsem("wgu0"), new_dsem("wgu1")]
            wdn_sem = [new_dsem("wdn0"), new_dsem("wdn1")]
            h2_sem = new_dsem("h2")
            out_sems = [new_dsem(f"out{i}") for i in range(4)]
            B_out = Buf("out")
            for blk in range(NB):
                t0 = blk * BT
                T.dma("sp", [lambda h, t0=t0: h.dma_start(out=h2blk, in_=h2T_d[:, :, t0:t0 + BT])], h2_sem, writes=[B_h2blk])
                T.op("dve", lambda h: h.memset(acc, 0.0), writes=B_acc)
                items = [(e, t4) for e in range(n_exp) for t4 in range(n512)]

                def gu(i):
                    e, t4 = items[i]
                    ee = e if n_exp == NE else (e if e < n_exp - 1 else NE - 1)
                    sl = e % 2
                    asl = i % 2
                    if t4 == 0:
                        T.dma("pool", [
                            lambda h: h.dma_start(out=wgu[sl][:, :, 0:256], in_=wg_d[ee].rearrange("(kc p) n -> p kc n", p=128)),
                            lambda h: h.dma_start(out=wgu[sl][:, :, 256:512], in_=wu_d[ee].rearrange("(kc p) n -> p kc n", p=128)),
                        ], wgu_sem[sl], writes=[B_wgu[sl]])
                        T.dma("pool", [lambda h: h.dma_start(out=wdn[sl], in_=wd_d[ee].rearrange("(fc p) n -> p fc n", p=128))],
                              wdn_sem[sl], writes=[B_wdn[sl]])
                    ts_ = slice(t4 * 512, (t4 + 1) * 512)
                    for fc in range(2):
                        ps_g, pb_g = next_ps()
                        ps_u, pb_u = next_ps()

                        def mmg(h, ps_g=ps_g, fc=fc):
                            for kc in range(8):
                                r = h.matmul(ps_g, wgu[sl][:, kc, fc * 128:(fc + 1) * 128], h2blk[:, kc, ts_], start=(kc == 0), stop=(kc == 7))
                            return r

                        def mmu(h, ps_u=ps_u, fc=fc):
                            for kc in range(8):
                                r = h.matmul(ps_u, wgu[sl][:, kc, 256 + fc * 128:256 + (fc + 1) * 128], h2blk[:, kc, ts_], start=(kc == 0), stop=(kc == 7))
                            return r
                        T.op("pe", mmg, reads=[B_wgu[sl], B_h2blk], writes=[pb_g])
                        T.op("pe", mmu, reads=[B_wgu[sl], B_h2blk], writes=[pb_u])
                        T.op("act", lambda h, ps_g=ps_g, fc=fc: h.activation(out=sgt[fc], in_=ps_g, func=AF.Silu), reads=[pb_g], writes=[B_sgt[fc]])
                        T.op("dve", lambda h, ps_u=ps_u, fc=fc: h.tensor_tensor(
                            out=actT[asl][:, fc, :], in0=ps_u, in1=sgt[fc], op=ALU.mult), reads=[pb_u, B_sgt[fc]], writes=[B_actT[asl]])

                def dn(i):
                    e, t4 = items[i]
                    ee = e if n_exp == NE else (e if e < n_exp - 1 else NE - 1)
                    sl = e % 2
                    asl = i % 2
                    for sub in range(4):
                        ti = t4 * 4 + sub
                        gcol = G_all[:, blk * nsub + ti, ee:ee + 1]
                        for hf in range(2):
                            ps_d, pb_d = next_ps()

                            def mmd(h, ps_d=ps_d, sub=sub, hf=hf):
                                for fc in range(2):
                                    r = h.matmul(ps_d, actT[asl][:, fc, sub * 128:(sub + 1) * 128], wdn[sl][:, fc, hf * 512:(hf + 1) * 512],
                                                 start=(fc == 0), stop=(fc == 1))
                                return r
                            T.op("pe", mmd, reads=[B_actT[asl], B_wdn[sl]], writes=[pb_d])
                            T.op("dve", lambda h, ps_d=ps_d, ti=ti, hf=hf, gcol=gcol: h.scalar_tensor_tensor(
                                out=acc[:, ti, hf * 512:(hf + 1) * 512], in0=ps_d, scalar=gcol, in1=acc[:, ti, hf * 512:(hf + 1) * 512],
                                op0=ALU.mult, op1=ALU.add), reads=[pb_d, B_G, B_acc[ti]], writes=[B_acc[ti]])

                gu(0)
                for i in range(len(items)):
                    if i + 1 < len(items):
                        gu(i + 1)
                    dn(i)
                if blk == 0:
                    tap("ffn1", acc[:, 1, :], B_acc[1])
                for ti in range(nsub):
                    tok0 = t0 + ti * 128
                    sl = ti % 2
                    T.dma("sp", [lambda h, sl=sl, tok0=tok0: h.dma_start(out=xt[sl], in_=x1_d[tok0:tok0 + 128, :])], x_sem[sl], writes=[B_xt[sl]])
                    a = acc[:, ti, :]
                    T.op("dve", lambda h, a=a: h.tensor_tensor(out=a, in0=a, in1=g2b, op=ALU.mult), reads=[B_acc[ti], B_g2b], writes=[B_acc[ti]])
                    T.op("dve", lambda h, a=a, sl=sl: h.scalar_tensor_tensor(out=a, in0=xt[sl], scalar=float(ALPHA), in1=a, op0=ALU.mult, op1=ALU.add),
                         reads=[B_xt[sl], B_acc[ti]], writes=[B_acc[ti]])
                    ln_stats(a, B_acc[ti], st6, mv, mv[:, 3:4], B_small)
                    T.op("dve", lambda h, a=a: h.tensor_scalar(out=a, in0=a, scalar1=mv[:, 0:1], scalar2=mv[:, 3:4], op0=ALU.subtract, op1=ALU.mult),
                         reads=[B_acc[ti], B_small], writes=[B_acc[ti]])
                    T.op("dve", lambda h, a=a: h.tensor_tensor(out=a, in0=a, in1=rowv[:, R_L2G:R_L2G + D], op=ALU.mult), reads=[B_acc[ti], B_rowv], writes=[B_acc[ti]])
                    T.op("dve", lambda h, a=a: h.tensor_tensor(out=a, in0=a, in1=rowv[:, R_L2B:R_L2B + D], op=ALU.add), reads=[B_acc[ti], B_rowv], writes=[B_acc[ti]])
                    T.dma("sp", [lambda h, a=a, tok0=tok0: h.dma_start(out=out_d[tok0:tok0 + 128, :], in_=a)], out_sems[ti % 4], reads=[B_acc[ti]], writes=[B_out])
                T.barrier()
        if do_moe:
            phase_B()
        T.barrier(final=True)
        with nc.Block() as block:
            T.emit(block)
    return nc, tap_out


def _host_pack(inputs, b):
    f = np.float32
    g = lambda k: np.asarray(inputs[k], dtype=f)
    consts = np.zeros((128, NCONST), f)
    consts[:, K_ID:K_ID + 128] = np.eye(128, dtype=f)
    jj, ll = np.meshgrid(np.arange(128), np.arange(128), indexing="ij")
    consts[:, K_TRIU:K_TRIU + 128] = (jj <= ll)
    consts[:, K_STRIL:K_STRIL + 128] = (jj > ll)
    consts[:, K_ONES:K_ONES + 128] = 1.0
    consts[:, K_EPSLN] = LN_EPS
    consts[:, K_EPSRMS] = RMS_EPS
    consts[:, K_ONE] = 1.0
    rowv = np.concatenate([g("ssd_dt_bias")[0], g("ssd_A_log")[0], g("ssd_D")[0], g("router_bias")[0], g("ssd_norm_w")[0],
                           g("ln1_g")[0], g("ln1_b")[0], g("ln2_g")[0], g("ln2_b")[0]])[None, :]
    assert rowv.shape[1] == NROW
    cw = g("ssd_conv_w")[0].reshape(4, 32, 128).transpose(2, 1, 0).reshape(128, 128)
    cb = g("ssd_conv_b")[0].reshape(32, 128).T
    scw = g("sc_conv_w")[0].reshape(3, 8, 128).transpose(2, 1, 0).reshape(128, 24)
    colv = np.ascontiguousarray(np.concatenate([cw, cb, scw], axis=1))
    assert colv.shape[1] == NCOL
    m = {
        "x": np.ascontiguousarray(g("x")[b]),
        "cT": np.ascontiguousarray(g("c")[b].reshape(8, 128).T),
        "w_ada": g("w_ada")[0], "b_ada": g("b_ada")[0][None, :], "w_in": g("w_in")[0],
        "colv": colv, "rowv": np.ascontiguousarray(rowv), "consts": consts,
        "w_ssd_out": g("w_ssd_out")[0], "w_sc_out": g("w_sc_out")[0], "w_o": g("w_o")[0], "router_w": g("router_w")[0],
    }
    return m


_SHARED = {}


def _shared_pack(inputs):
    f = np.float32
    g = lambda k: np.asarray(inputs[k], dtype=f)
    return {
        "w_gate": np.concatenate([g("w_gate")[0], g("sh_gate")], axis=0),
        "w_up": np.concatenate([g("w_up")[0], g("sh_up")], axis=0),
        "w_down": np.concatenate([g("w_down")[0], g("sh_down")], axis=0),
    }


def kernel(**inputs):
    nc, _ = build()
    shared = _shared_pack(inputs)
    in_maps = []
    for b in range(8):
        m = _host_pack(inputs, b)
        m.update(shared)
        in_maps.append(m)
    res = run_bass_kernel_spmd(nc, in_maps, core_ids=list(range(8)))
    return np.stack([np.asarray(r["out"], dtype=np.float32) for r in res.results], axis=0)
```
